# Optimizing a Trainium2 kernel written in Bass

```python
import math
import jax, jax.numpy as jnp
from jax import lax
import numpy as np

D_MODEL = 2048
BATCH = 8
SEQ = 2048
DEPTH = 2

CTX_LEN = 256
GRID_W = 64
HEAD_DIM = 128
ROPE_THETA = 10000.0
Q_BLOCK = 128
EPS = 1e-6
DIFF_HEADS = 4
DIFF_V_DIM = 2 * HEAD_DIM
LRU_WIDTH = 1024
LRU_BLOCKS = 8
LRU_BLOCK_W = LRU_WIDTH // LRU_BLOCKS
CONV_W = 4
LRU_C = 8.0
GQA_HEADS = 8
GQA_KV_HEADS = 2
N_BRANCH = 3
BRANCH_W = 1024
N_GROUPS = 4
EXPERTS_PER_GROUP = 8
N_EXPERTS = N_GROUPS * EXPERTS_PER_GROUP
TOP_K = 2
D_EXPERT = 512
N_MOD = 6

A_Q = 2 * DIFF_HEADS * HEAD_DIM
A_K = 2 * DIFF_HEADS * HEAD_DIM
A_V = DIFF_HEADS * DIFF_V_DIM
C_Q = GQA_HEADS * HEAD_DIM
C_KV = GQA_KV_HEADS * HEAD_DIM
SPLITS = (A_Q, A_K, A_V, LRU_WIDTH, LRU_WIDTH, C_Q, C_KV, C_KV, N_BRANCH * D_MODEL)
IN_COLS = A_Q + A_K + A_V + 2 * LRU_WIDTH + C_Q + 2 * C_KV + N_BRANCH * D_MODEL

kernel_name = "hybrid_diff_rglru_gqa_hmoe_dit"

F32 = jnp.float32


def rms_norm(x, g):
    xf = x.astype(F32)
    y = xf * lax.rsqrt(jnp.mean(xf * xf, axis=-1, keepdims=True) + EPS)
    return (y * g.astype(F32)).astype(x.dtype)


def modulate(x, shift, scale):
    return x * (1 + scale) + shift


def axial_rope_tables(n_tokens):
    n_rows = n_tokens // GRID_W
    row = jnp.repeat(jnp.arange(n_rows), GRID_W).astype(F32)
    col = jnp.tile(jnp.arange(GRID_W), n_rows).astype(F32)
    half = HEAD_DIM // 2
    inv = 1.0 / (ROPE_THETA ** (jnp.arange(0, half, 2, dtype=F32) / half))
    ang_r = row[:, None] * inv
    ang_c = col[:, None] * inv
    ang = jnp.concatenate([ang_r, ang_r, ang_c, ang_c], axis=-1)
    return jnp.cos(ang), jnp.sin(ang)


def _rot_half(u):
    u1, u2 = jnp.split(u, 2, axis=-1)
    return jnp.concatenate([-u2, u1], axis=-1)


def apply_axial_rope(x, cos, sin):
    xf = x.astype(F32)
    xr, xc = jnp.split(xf, 2, axis=-1)
    rot = jnp.concatenate([_rot_half(xr), _rot_half(xc)], axis=-1)
    return (xf * cos[:, None, :] + rot * sin[:, None, :]).astype(x.dtype)


def attn_probs(q, k):
    s = jnp.einsum('bqhgd,bkhd->bhgqk', q, k, preferred_element_type=F32) * (HEAD_DIM ** -0.5)
    return jax.nn.softmax(s, axis=-1)


def diff_attention(q, k, v, lam, lambda_init, sub_g):
    b, tq = q.shape[:2]
    p = attn_probs(q.reshape(b, tq, 2 * DIFF_HEADS, 1, HEAD_DIM), k)[:, :, 0]
    p = p.reshape(b, DIFF_HEADS, 2, tq, p.shape[-1])
    w = p[:, :, 0] - lam * p[:, :, 1]
    o = jnp.einsum('bhqk,bkhe->bqhe', w.astype(v.dtype), v)
    o = rms_norm(o, sub_g) * (1.0 - lambda_init)
    return o.reshape(b, tq, DIFF_HEADS * DIFF_V_DIM)


def gqa_attention(q, k, v):
    b, tq = q.shape[:2]
    qg = q.reshape(b, tq, GQA_KV_HEADS, GQA_HEADS // GQA_KV_HEADS, HEAD_DIM)
    p = attn_probs(qg, k)
    o = jnp.einsum('bhgqk,bkhd->bqhgd', p.astype(v.dtype), v)
    return o.reshape(b, tq, GQA_HEADS * HEAD_DIM)


def sweep_query_blocks(q, fn):
    b, t = q.shape[:2]
    nb = t // Q_BLOCK
    qb = q.reshape(b, nb, Q_BLOCK, *q.shape[2:]).swapaxes(0, 1)
    out = lax.map(fn, qb)
    return out.swapaxes(0, 1).reshape(b, t, *out.shape[3:])


def centred_depthwise_conv(x, w, bias):
    t = x.shape[1]
    left = CONV_W // 2
    right = CONV_W - 1 - left
    xp = jnp.pad(x, ((0, 0), (left, right), (0, 0)))
    out = xp[:, 0:t] * w[0]
    for k in range(1, CONV_W):
        out = out + xp[:, k:k + t] * w[k]
    return out + bias


def block_diag_linear(x, w, b):
    xb = x.reshape(*x.shape[:-1], LRU_BLOCKS, LRU_BLOCK_W)
    y = jnp.einsum('btnc,ncd->btnd', xb, w)
    return y.reshape(x.shape) + b


def linear_scan(a, b, h0, reverse):
    def combine(e1, e2):
        a1, b1 = e1
        a2, b2 = e2
        return a1 * a2, a2 * b1 + b2
    a_cum, h = lax.associative_scan(combine, (a, b), axis=1, reverse=reverse)
    return h + a_cum * h0[:, None, :]


def rglru_scan(xc, h0, w_a, b_a, w_x, b_x, lam, reverse):
    r = jax.nn.sigmoid(block_diag_linear(xc, w_a, b_a).astype(F32))
    i = jax.nn.sigmoid(block_diag_linear(xc, w_x, b_x).astype(F32))
    log_a = -LRU_C * r * jax.nn.softplus(-lam.astype(F32))
    a = jnp.exp(log_a)
    mult = jnp.sqrt(-jnp.expm1(2.0 * log_a))
    return linear_scan(a, mult * i * xc.astype(F32), h0, reverse)


def merge_branches(ya, yb, yc, gate_logits, b_gate, w_branch_a, w_branch_b, w_branch_c, w_out):
    g = jax.nn.sigmoid((gate_logits.reshape(*gate_logits.shape[:-1], N_BRANCH, D_MODEL) + b_gate).astype(F32)).astype(ya.dtype)
    m = g[..., 0, :] * (ya @ w_branch_a) + g[..., 1, :] * (yb @ w_branch_b) + g[..., 2, :] * (yc @ w_branch_c)
    return m @ w_out


def stream_projections(u, rope, w_in, q_norm_a, k_norm_a, q_norm_c, k_norm_c):
    b, t, _ = u.shape
    idx = np.cumsum(np.array(SPLITS))[:-1].tolist()
    aq, ak, av, bx, by, cq, ck, cv, gate_logits = jnp.split(u @ w_in, idx, axis=-1)
    aq = rms_norm(aq.reshape(b, t, 2 * DIFF_HEADS, HEAD_DIM), q_norm_a)
    ak = rms_norm(ak.reshape(b, t, 2 * DIFF_HEADS, HEAD_DIM), k_norm_a)
    cq = rms_norm(cq.reshape(b, t, GQA_HEADS, HEAD_DIM), q_norm_c)
    ck = rms_norm(ck.reshape(b, t, GQA_KV_HEADS, HEAD_DIM), k_norm_c)
    if rope is not None:
        cos, sin = rope
        aq = apply_axial_rope(aq, cos, sin)
        ak = apply_axial_rope(ak, cos, sin)
        cq = apply_axial_rope(cq, cos, sin)
        ck = apply_axial_rope(ck, cos, sin)
    av = av.reshape(b, t, DIFF_HEADS, DIFF_V_DIM)
    cv = cv.reshape(b, t, GQA_KV_HEADS, HEAD_DIM)
    return aq, ak, av, bx, by, cq, ck, cv, gate_logits


def mixer_sublayer(u_lat, u_ctx, rope, lambda_init, need_ctx_out, w_in, b_gate, q_norm_a, k_norm_a,
                   diff_lambda, sub_norm_a, q_norm_c, k_norm_c, conv_w, conv_b, lru_w_a, lru_b_a,
                   lru_w_x, lru_b_x, lru_lambda, w_branch_a, w_branch_b, w_branch_c, w_out):
    lam = (jnp.exp(jnp.sum(diff_lambda[0] * diff_lambda[1]).astype(F32))
           - jnp.exp(jnp.sum(diff_lambda[2] * diff_lambda[3]).astype(F32)) + lambda_init)
    aq_x, ak_x, av_x, bx_x, by_x, cq_x, ck_x, cv_x, gl_x = stream_projections(
        u_ctx, None, w_in, q_norm_a, k_norm_a, q_norm_c, k_norm_c)
    aq_l, ak_l, av_l, bx_l, by_l, cq_l, ck_l, cv_l, gl_l = stream_projections(
        u_lat, rope, w_in, q_norm_a, k_norm_a, q_norm_c, k_norm_c)

    xc_x = centred_depthwise_conv(bx_x, conv_w, conv_b)
    xc_l = centred_depthwise_conv(bx_l, conv_w, conv_b)
    zeros = jnp.zeros((u_ctx.shape[0], LRU_WIDTH), F32)
    hf_x = rglru_scan(xc_x, zeros, lru_w_a[0], lru_b_a[0], lru_w_x[0], lru_b_x[0], lru_lambda[0], False)
    hb_x = rglru_scan(xc_x, zeros, lru_w_a[1], lru_b_a[1], lru_w_x[1], lru_b_x[1], lru_lambda[1], True)
    hf_l = rglru_scan(xc_l, hf_x[:, -1], lru_w_a[0], lru_b_a[0], lru_w_x[0], lru_b_x[0], lru_lambda[0], False)
    hb_l = rglru_scan(xc_l, hb_x[:, 0], lru_w_a[1], lru_b_a[1], lru_w_x[1], lru_b_x[1], lru_lambda[1], True)
    yb_l = (hf_l + hb_l).astype(u_lat.dtype) * jax.nn.gelu(by_l)

    ka = jnp.concatenate([ak_x, ak_l], axis=1)
    va = jnp.concatenate([av_x, av_l], axis=1)
    ya_l = sweep_query_blocks(aq_l, lambda qb: diff_attention(qb, ka, va, lam, lambda_init, sub_norm_a))
    kc = jnp.concatenate([ck_x, ck_l], axis=1)
    vc = jnp.concatenate([cv_x, cv_l], axis=1)
    yc_l = sweep_query_blocks(cq_l, lambda qb: gqa_attention(qb, kc, vc))
    y_lat = merge_branches(ya_l, yb_l, yc_l, gl_l, b_gate, w_branch_a, w_branch_b, w_branch_c, w_out)
    if not need_ctx_out:
        return y_lat, None
    ya_x = diff_attention(aq_x, ak_x, av_x, lam, lambda_init, sub_norm_a)
    yc_x = gqa_attention(cq_x, ck_x, cv_x)
    yb_x = (hf_x + hb_x).astype(u_ctx.dtype) * jax.nn.gelu(by_x)
    y_ctx = merge_branches(ya_x, yb_x, yc_x, gl_x, b_gate, w_branch_a, w_branch_b, w_branch_c, w_out)
    return y_lat, y_ctx


def hierarchical_moe(t, w_group, b_group, w_route, b_route, w1, w3, w2):
    n = t.shape[0]
    gl = (t @ w_group).astype(F32) + b_group
    gp = jax.nn.softmax(gl, axis=-1)
    g_idx = jnp.argmax(gl, axis=-1)
    g_w = jnp.take_along_axis(gp, g_idx[:, None], axis=-1)
    el = ((t @ w_route).astype(F32) + b_route).reshape(n, N_GROUPS, EXPERTS_PER_GROUP)
    el_sel = jnp.take_along_axis(el, g_idx[:, None, None], axis=1)[:, 0]
    top_v, top_i = lax.top_k(el_sel, TOP_K)
    top_p = jax.nn.softmax(top_v, axis=-1) * g_w
    expert_id = g_idx[:, None] * EXPERTS_PER_GROUP + top_i
    combine = jnp.sum(jax.nn.one_hot(expert_id, N_EXPERTS, dtype=F32) * top_p[..., None], axis=1).astype(t.dtype)
    out = jnp.zeros_like(t)
    for e in range(N_EXPERTS):
        h = jax.nn.silu(t @ w1[e]) * (t @ w3[e])
        out = out + combine[:, e:e + 1] * (h @ w2[e])
    return out


def setup_inputs(seed: int = 0) -> dict:
    key = jax.random.key(seed)
    ks = list(jax.random.split(key, 40))

    def nrm(i, shape, scale):
        return jax.random.normal(ks[i], shape, F32) * scale

    L, D = DEPTH, D_MODEL
    u = jax.random.uniform(ks[39], (L, 2, LRU_WIDTH), F32, minval=0.9, maxval=0.999)
    a_base = u ** (1.0 / LRU_C)
    lru_lambda = jnp.log(a_base) - jnp.log1p(-a_base)
    return {
        "x": nrm(0, (BATCH, SEQ, D), 1.0),
        "c": nrm(1, (BATCH, D), 1.0),
        "ctx": nrm(2, (BATCH, CTX_LEN, D), 1.0),
        "c_ctx": nrm(3, (D,), 1.0),
        "w_ada": nrm(4, (L, D, N_MOD * D), 0.5 * D ** -0.5),
        "b_ada": nrm(5, (L, N_MOD * D), 0.01),
        "norm1_g": 1.0 + nrm(6, (L, D), 0.02),
        "norm2_g": 1.0 + nrm(7, (L, D), 0.02),
        "w_in": nrm(8, (L, D, IN_COLS), D ** -0.5),
        "b_gate": nrm(9, (L, N_BRANCH, D), 0.01),
        "q_norm_a": 1.0 + nrm(10, (L, HEAD_DIM), 0.02),
        "k_norm_a": 1.0 + nrm(11, (L, HEAD_DIM), 0.02),
        "diff_lambda": nrm(12, (L, 4, HEAD_DIM), 0.1),
        "sub_norm_a": 1.0 + nrm(13, (L, DIFF_V_DIM), 0.02),
        "q_norm_c": 1.0 + nrm(14, (L, HEAD_DIM), 0.02),
        "k_norm_c": 1.0 + nrm(15, (L, HEAD_DIM), 0.02),
        "conv_w": nrm(16, (L, CONV_W, LRU_WIDTH), CONV_W ** -0.5),
        "conv_b": nrm(17, (L, LRU_WIDTH), 0.01),
        "lru_w_a": nrm(18, (L, 2, LRU_BLOCKS, LRU_BLOCK_W, LRU_BLOCK_W), LRU_BLOCK_W ** -0.5),
        "lru_b_a": nrm(19, (L, 2, LRU_WIDTH), 0.01),
        "lru_w_x": nrm(20, (L, 2, LRU_BLOCKS, LRU_BLOCK_W, LRU_BLOCK_W), LRU_BLOCK_W ** -0.5),
        "lru_b_x": nrm(21, (L, 2, LRU_WIDTH), 0.01),
        "lru_lambda": lru_lambda,
        "w_branch_a": nrm(22, (L, BRANCH_W, D), BRANCH_W ** -0.5),
        "w_branch_b": nrm(23, (L, BRANCH_W, D), BRANCH_W ** -0.5),
        "w_branch_c": nrm(24, (L, BRANCH_W, D), BRANCH_W ** -0.5),
        "w_out": nrm(25, (L, D, D), D ** -0.5),
        "w_group": nrm(26, (L, D, N_GROUPS), D ** -0.5),
        "b_group": nrm(27, (L, N_GROUPS), 0.01),
        "w_route": nrm(28, (L, D, N_EXPERTS), D ** -0.5),
        "b_route": nrm(29, (L, N_EXPERTS), 0.01),
        "w1": nrm(30, (L, N_EXPERTS, D, D_EXPERT), D ** -0.5),
        "w3": nrm(31, (L, N_EXPERTS, D, D_EXPERT), D ** -0.5),
        "w2": nrm(32, (L, N_EXPERTS, D_EXPERT, D), D_EXPERT ** -0.5),
    }


def reference(x, c, ctx, c_ctx, w_ada, b_ada, norm1_g, norm2_g, w_in, b_gate, q_norm_a, k_norm_a,
              diff_lambda, sub_norm_a, q_norm_c, k_norm_c, conv_w, conv_b, lru_w_a, lru_b_a,
              lru_w_x, lru_b_x, lru_lambda, w_branch_a, w_branch_b, w_branch_c, w_out,
              w_group, b_group, w_route, b_route, w1, w3, w2):
    b, t, d = x.shape
    rope = axial_rope_tables(t)
    silu_c = jax.nn.silu(c)
    silu_cc = jax.nn.silu(c_ctx)
    h_lat, h_ctx = x, ctx
    for l in range(DEPTH):
        last = l == DEPTH - 1
        lambda_init = 0.8 - 0.6 * math.exp(-0.3 * l)
        mod_l = (silu_c @ w_ada[l] + b_ada[l]).reshape(b, N_MOD, 1, d)
        mod_x = (silu_cc @ w_ada[l] + b_ada[l]).reshape(N_MOD, d)
        u_lat = modulate(rms_norm(h_lat, norm1_g[l]), mod_l[:, 0], mod_l[:, 1])
        u_ctx = modulate(rms_norm(h_ctx, norm1_g[l]), mod_x[0], mod_x[1])
        y_lat, y_ctx = mixer_sublayer(
            u_lat, u_ctx, rope, lambda_init, not last, w_in[l], b_gate[l], q_norm_a[l], k_norm_a[l],
            diff_lambda[l], sub_norm_a[l], q_norm_c[l], k_norm_c[l], conv_w[l], conv_b[l],
            lru_w_a[l], lru_b_a[l], lru_w_x[l], lru_b_x[l], lru_lambda[l],
            w_branch_a[l], w_branch_b[l], w_branch_c[l], w_out[l])
        h_lat = h_lat + mod_l[:, 2] * y_lat
        v_lat = modulate(rms_norm(h_lat, norm2_g[l]), mod_l[:, 3], mod_l[:, 4])
        if last:
            out = hierarchical_moe(v_lat.reshape(-1, d), w_group[l], b_group[l], w_route[l], b_route[l],
                                   w1[l], w3[l], w2[l])
            h_lat = h_lat + mod_l[:, 5] * out.reshape(b, t, d)
        else:
            h_ctx = h_ctx + mod_x[2] * y_ctx
            v_ctx = modulate(rms_norm(h_ctx, norm2_g[l]), mod_x[3], mod_x[4])
            n_lat = b * t
            tokens = jnp.concatenate([v_lat.reshape(-1, d), v_ctx.reshape(-1, d)], axis=0)
            out = hierarchical_moe(tokens, w_group[l], b_group[l], w_route[l], b_route[l],
                                   w1[l], w3[l], w2[l])
            h_lat = h_lat + mod_l[:, 5] * out[:n_lat].reshape(b, t, d)
            h_ctx = h_ctx + mod_x[5] * out[n_lat:].reshape(h_ctx.shape)
    return h_lat
```

```python
import math
import numpy as np
from contextlib import ExitStack
import concourse.bass as bass
import concourse.mybir as mybir
from concourse.bass_utils import run_bass_kernel_spmd

F32 = mybir.dt.float32
BF16 = mybir.dt.bfloat16
U8 = mybir.dt.uint8
AF = mybir.ActivationFunctionType
ALU = mybir.AluOpType
AX = mybir.AxisListType

ENGS = ['sync', 'act', 'dve', 'pool', 'pe']
NDMA = 8
EPOCH = 30000


class Op:
    __slots__ = ('eng', 'fn', 'deps', 'is_dma', 'sig', 'sigval', 'name')


class Sched:
    def __init__(self):
        self.ops = {e: [] for e in ENGS}
        self.tok_w = {}
        self.tok_r = {}
        self.last = {e: None for e in ENGS}
        self.dmas = {e: [] for e in ENGS}

    def add(self, eng, fn, reads=(), writes=(), dma=False, name=None):
        op = Op()
        op.eng = eng
        op.fn = fn
        op.is_dma = dma
        op.sig = dma
        op.sigval = None
        op.name = name
        deps = {}
        for t in reads:
            w = self.tok_w.get(t)
            if w is not None:
                deps[id(w)] = (w, 'raw')
        for t in writes:
            w = self.tok_w.get(t)
            if w is not None and id(w) not in deps:
                deps[id(w)] = (w, 'waw')
            for r in self.tok_r.get(t, ()):
                if id(r) not in deps:
                    deps[id(r)] = (r, 'war')
        dl = []
        for d, kind in deps.values():
            if (not d.is_dma) and (not dma) and d.eng == eng:
                if eng == 'pe':
                    continue
                if kind != 'raw':
                    continue
            dl.append(d)
        op.deps = dl
        for t in reads:
            self.tok_r.setdefault(t, []).append(op)
        for t in writes:
            self.tok_w[t] = op
            self.tok_r[t] = []
        self.ops[eng].append(op)
        if dma:
            self.dmas[eng].append(op)
        else:
            self.last[eng] = op
        return op

    def barrier(self):
        deps = []
        for e in ENGS:
            if self.last[e] is not None:
                deps.append(self.last[e])
            deps.extend(self.dmas[e][-NDMA:])
        for e in ENGS:
            op = Op()
            op.eng = e
            op.fn = None
            op.is_dma = False
            op.sig = False
            op.sigval = None
            op.name = 'barrier'
            op.deps = list(deps)
            self.ops[e].append(op)
        self.tok_w = {}
        self.tok_r = {}

    def emit(self, nc, stack):
        for e in ENGS:
            for op in self.ops[e]:
                for d in op.deps:
                    d.sig = True
        csem = {}
        for e in ENGS:
            n = sum(1 for op in self.ops[e] if op.sig and not op.is_dma)
            csem[e] = [stack.enter_context(nc.semaphore('c_%s_%d' % (e, i)))
                       for i in range(n // EPOCH + 1)]
        dsem = {}
        for e in ENGS:
            if self.dmas[e]:
                dsem[e] = [stack.enter_context(nc.semaphore('d_%s_%d' % (e, i)))
                           for i in range(NDMA)]
        for e in ENGS:
            k = 0
            j = 0
            for op in self.ops[e]:
                if op.is_dma:
                    op.sigval = (dsem[e][j % NDMA], 16 * (j // NDMA + 1), ('d', e, j % NDMA))
                    j += 1
                elif op.sig:
                    op.sigval = (csem[e][k // EPOCH], k % EPOCH + 1, ('c', e, k // EPOCH))
                    k += 1
        blk = stack.enter_context(nc.Block())
        hooks = {'sync': blk.sync, 'act': blk.scalar, 'dve': blk.vector,
                 'pool': blk.gpsimd, 'pe': blk.tensor}
        all_sigs = []
        for e in ENGS:
            for op in self.ops[e]:
                if op.sigval is not None:
                    all_sigs.append(op)

        def make(e):
            def body(eng):
                waited = {}

                def wait(sv):
                    sem, val, key = sv
                    if waited.get(key, 0) < val:
                        eng.wait_ge(sem, val)
                        waited[key] = val
                for op in self.ops[e]:
                    for d in op.deps:
                        wait(d.sigval)
                    if op.is_dma:
                        sem, val, key = op.sigval
                        if val > 16:
                            wait((sem, val - 16, key))
                    if op.fn is not None:
                        ins = op.fn(eng)
                        if op.sigval is not None:
                            sem, val, key = op.sigval
                            ins.then_inc(sem, 16 if op.is_dma else 1)
                if e == 'sync':
                    final = {}
                    for op in all_sigs:
                        sem, val, key = op.sigval
                        if key not in final or final[key][1] < val:
                            final[key] = (sem, val, key)
                    for sv in final.values():
                        wait(sv)
            return body
        for e in ENGS:
            hooks[e](make(e))


class Arena:
    def __init__(self, nc, stack, nbytes, name='arena'):
        self.t = stack.enter_context(nc.sbuf_tensor(name, [128, nbytes], U8))
        self.nbytes = nbytes
        self.off = 0
        self.marks = []
        self.peak = 0

    def alloc(self, shape, dtype, parts=128):
        esz = 2 if dtype == BF16 else 4
        n = int(np.prod(shape))
        nb = n * esz
        off = (self.off + 31) // 32 * 32
        assert off + nb <= self.nbytes, ('arena overflow', off + nb, self.nbytes)
        self.off = off + nb
        self.peak = max(self.peak, self.off)
        v = self.t[0:parts, off:off + nb].bitcast(dtype)
        if len(shape) > 1:
            names = ' '.join('d%d' % i for i in range(len(shape)))
            kw = {'d%d' % i: int(shape[i]) for i in range(len(shape))}
            v = v.rearrange('p (%s) -> p %s' % (names, names), **kw)
        return v

    def mark(self):
        self.marks.append(self.off)

    def release(self):
        self.off = self.marks.pop()


D = 2048
KD = 16
TC = 256
TL = 2048
T = TC + TL
NT = T // 128
DEPTH = 2
HD = 128
EPS = 1e-6
NEXP = 32
DEXP = 512
IN_COLS = 12800
BLKS = [(0, 256), (256, 512), (768, 512), (1280, 512), (1792, 512)]

F_N1G, F_N2G, F_BG, F_CW, F_CB = 0, 16, 32, 80, 112
F_BA, F_BX, F_LAM, F_QNA, F_KNA, F_QNC, F_KNC, F_SUB = 128, 144, 160, 176, 177, 178, 179, 180

WEIGHT_NAMES = ["w_ada", "b_ada", "norm1_g", "norm2_g", "w_in", "b_gate", "q_norm_a", "k_norm_a",
                "diff_lambda", "sub_norm_a", "q_norm_c", "k_norm_c", "conv_w", "conv_b", "lru_w_a",
                "lru_b_a", "lru_w_x", "lru_b_x", "lru_lambda", "w_branch_a", "w_branch_b",
                "w_branch_c", "w_out", "w_group", "b_group", "w_route", "b_route", "w1", "w3", "w2"]
WEIGHT_SHAPES = {
    "w_ada": [2, 2048, 12288], "b_ada": [2, 12288], "norm1_g": [2, 2048], "norm2_g": [2, 2048],
    "w_in": [2, 2048, 12800], "b_gate": [2, 3, 2048], "q_norm_a": [2, 128], "k_norm_a": [2, 128],
    "diff_lambda": [2, 4, 128], "sub_norm_a": [2, 256], "q_norm_c": [2, 128], "k_norm_c": [2, 128],
    "conv_w": [2, 4, 1024], "conv_b": [2, 1024], "lru_w_a": [2, 2, 8, 128, 128], "lru_b_a": [2, 2, 1024],
    "lru_w_x": [2, 2, 8, 128, 128], "lru_b_x": [2, 2, 1024], "lru_lambda": [2, 2, 1024],
    "w_branch_a": [2, 1024, 2048], "w_branch_b": [2, 1024, 2048], "w_branch_c": [2, 1024, 2048],
    "w_out": [2, 2048, 2048], "w_group": [2, 2048, 4], "b_group": [2, 4], "w_route": [2, 2048, 32],
    "b_route": [2, 32], "w1": [2, 32, 2048, 512], "w3": [2, 32, 2048, 512], "w2": [2, 32, 512, 2048],
}


class _Stop(Exception):
    pass


def build(dbg=False, phases=None, nlayers=DEPTH, stop=None):
    nc = bass.Bass("TRN2", target_bir_lowering=False)
    IN = {}
    IN["x"] = nc.dram_tensor("x", [TL, D], F32, kind="ExternalInput").ap()
    IN["ctx"] = nc.dram_tensor("ctx", [TC, D], F32, kind="ExternalInput").ap()
    IN["c2"] = nc.dram_tensor("c2", [2, D], F32, kind="ExternalInput").ap()
    for n in WEIGHT_NAMES:
        IN[n] = nc.dram_tensor(n, WEIGHT_SHAPES[n], F32, kind="ExternalInput").ap()
    IN["ident"] = nc.dram_tensor("ident", [128, 128], F32, kind="ExternalInput").ap()
    IN["rotm"] = nc.dram_tensor("rotm", [128, 128], F32, kind="ExternalInput").ap()
    IN["cosT"] = nc.dram_tensor("cosT", [128, T], F32, kind="ExternalInput").ap()
    IN["sinT"] = nc.dram_tensor("sinT", [128, T], F32, kind="ExternalInput").ap()
    OUT = nc.dram_tensor("out", [TL, D], F32, kind="ExternalOutput").ap()
    sk = "ExternalOutput" if dbg else "Internal"

    def scratch(name, shape, dt):
        return nc.dram_tensor(name, shape, dt, kind=sk).ap()
    H = scratch("H", [T, D], F32)
    GATE = scratch("GATE", [DEPTH, 2, 2, D], F32)
    QA = scratch("QA", [8, 128, T], BF16)
    KA = scratch("KA", [8, 128, T], BF16)
    QC = scratch("QC", [8, 128, T], BF16)
    KC = scratch("KC", [2, 128, T], BF16)
    VA = scratch("VA", [T, 1024], BF16)
    VC = scratch("VC", [T, 256], BF16)
    BX = scratch("BX", [8, 128, T], F32)
    BYG = scratch("BYG", [8, 128, T], BF16)
    G = scratch("G", [48, 128, T], BF16)
    YA = scratch("YA", [8, 128, T], BF16)
    YB = scratch("YB", [8, 128, T], BF16)
    YC = scratch("YC", [8, 128, T], BF16)
    MT = scratch("MT", [16, 128, T], BF16)
    VT = scratch("VT", [16, 128, T], BF16)
    CMB = scratch("CMB", [NEXP, T], F32)

    S = Sched()
    uid = [0]

    def U(prefix):
        uid[0] += 1
        return (prefix, uid[0])

    def chk(name):
        if stop == name:
            raise _Stop()

    def body(st):
        ar = Arena(nc, st, 200 * 1024)
        psum = st.enter_context(nc.psum_tensor("ps", [128, 4096], F32))

        def bank(b, n=512, parts=128):
            return psum[0:parts, b * 512:b * 512 + n]

        def PS(b):
            return ('ps', b)

        def dma(q, out, in_, reads=(), writes=()):
            return S.add(q, lambda e: e.dma_start(out=out, in_=in_), reads, writes, dma=True)

        def act(out, in_, func, reads, writes, bias=None, scale=None, accum=None):
            kw = {}
            if bias is not None:
                kw['bias'] = bias
            if scale is not None:
                kw['scale'] = scale
            if accum is not None:
                kw['accum_out'] = accum
            return S.add('act', lambda e: e.activation(out=out, in_=in_, func=func, **kw), reads, writes)

        def tt(eng, out, a, b, op, reads, writes):
            return S.add(eng, lambda e: e.tensor_tensor(out=out, in0=a, in1=b, op=op), reads, writes)

        def ts(eng, out, a, s1, s2, op0, op1, reads, writes):
            if s2 is None:
                return S.add(eng, lambda e: e.tensor_scalar(out=out, in0=a, scalar1=s1, scalar2=None, op0=op0), reads, writes)
            return S.add(eng, lambda e: e.tensor_scalar(out=out, in0=a, scalar1=s1, scalar2=s2, op0=op0, op1=op1), reads, writes)

        def stt(out, a, s, b, op0, op1, reads, writes):
            return S.add('dve', lambda e: e.scalar_tensor_tensor(out=out, in0=a, scalar=s, in1=b, op0=op0, op1=op1), reads, writes)

        def cp(eng, out, in_, reads, writes):
            return S.add(eng, lambda e: e.tensor_copy(out=out, in_=in_), reads, writes)

        def recip(out, in_, reads, writes):
            return S.add('dve', lambda e: e.reciprocal(out=out, in_=in_), reads, writes)

        def mms(out, pairs, reads, writes):
            def f(e):
                n = len(pairs)
                ins = None
                for i, (l, r) in enumerate(pairs):
                    ins = e.matmul(out, l, r, start=(i == 0), stop=(i == n - 1))
                return ins
            return S.add('pe', f, reads, writes)

        def transposes(items, ident, reads, writes):
            def f(e):
                ins = None
                for (o, i_) in items:
                    ins = e.transpose(out=o, in_=i_, identity=ident)
                return ins
            return S.add('pe', f, reads, writes)

        ident_f = ar.alloc([128], F32)
        ident_b = ar.alloc([128], BF16)
        ones_b = ar.alloc([128], BF16)
        rotm = ar.alloc([128], BF16)
        featT = ar.alloc([DEPTH, 2, 128], F32)
        modT = ar.alloc([DEPTH, 96, 2], F32)
        AB = ar.alloc([DEPTH, 2, 4, 16], F32)
        lamv = ar.alloc([DEPTH, 4], F32)
        lruS = ar.alloc([DEPTH, 2, 16], F32)
        epsc = ar.alloc([4], F32)
        dma('sync', ident_f, IN["ident"], writes=['ident_f'])
        dma('pool', ident_b, IN["ident"], writes=['ident_b'])
        dma('pool', rotm, IN["rotm"], writes=['rotm'])
        S.add('pool', lambda e: e.memset(ones_b, 1.0), writes=['ones_b'])
        S.add('pool', lambda e: e.memset(epsc, EPS), writes=['epsc'])
        eps_ap = epsc[:, 0:1]

        ar.mark()
        stg = ar.alloc([2, 128], F32)
        for l in range(DEPTH):
            S.add('pool', lambda e: e.memset(stg, 0.0), writes=['stg'])
            def ld(off, n, src):
                tl, o = divmod(off, 128)
                dma('sync', stg[o:o + n, tl, :], src, reads=['stg'], writes=[U('stgp')])
            ld(F_N1G, 16, IN["norm1_g"][l].rearrange("(n p) -> n p", p=128))
            ld(F_N2G, 16, IN["norm2_g"][l].rearrange("(n p) -> n p", p=128))
            ld(F_BG, 48, IN["b_gate"][l].rearrange("a (n p) -> (a n) p", p=128))
            ld(F_CW, 32, IN["conv_w"][l].rearrange("a (n p) -> (a n) p", p=128))
            ld(F_CB, 8, IN["conv_b"][l].rearrange("(n p) -> n p", p=128))
            ld(F_BA, 16, IN["lru_b_a"][l].rearrange("a (n p) -> (a n) p", p=128))
            ld(F_BX, 16, IN["lru_b_x"][l].rearrange("a (n p) -> (a n) p", p=128))
            ld(F_LAM, 16, IN["lru_lambda"][l].rearrange("a (n p) -> (a n) p", p=128))
            ld(F_QNA, 1, IN["q_norm_a"][l:l + 1, :])
            ld(F_KNA, 1, IN["k_norm_a"][l:l + 1, :])
            ld(F_QNC, 1, IN["q_norm_c"][l:l + 1, :])
            ld(F_KNC, 1, IN["k_norm_c"][l:l + 1, :])
            ld(F_SUB, 2, IN["sub_norm_a"][l].rearrange("(n p) -> n p", p=128))
            S.barrier()
            transposes([(bank(0, 128), stg[:, 0, :]), (bank(1, 128), stg[:, 1, :])], ident_f,
                       reads=['stg', 'ident_f'], writes=[PS(0), PS(1)])
            cp('dve', featT[:, l, 0, :], bank(0, 128), reads=[PS(0)], writes=['featT'])
            cp('dve', featT[:, l, 1, :], bank(1, 128), reads=[PS(1)], writes=['featT'])
            S.barrier()
        ar.release()

        if dbg:
            FEAT = nc.dram_tensor('FEAT', [128, DEPTH * 2 * 128], F32, kind='ExternalOutput').ap()
            dma('sync', FEAT, featT.rearrange('p a b c -> p (a b c)'), writes=[U('FEAT')])
        chk('F')

        def FT(l, ch):
            tl, o = divmod(ch, 128)
            return featT[:, l, tl, o:o + 1]

        ar.mark()
        c2t = ar.alloc([D], F32, parts=2)
        sct = ar.alloc([D], F32, parts=2)
        sc2 = ar.alloc([KD, 2], BF16)
        brow = ar.alloc([6 * D], F32, parts=2)
        modrow = ar.alloc([6 * D], F32, parts=2)
        wa = [ar.alloc([KD, 512], BF16) for _ in range(3)]
        dl = ar.alloc([4, 128], F32)
        pr = ar.alloc([2, 128], F32)
        sm = ar.alloc([2], F32)
        dma('sync', c2t, IN["c2"], writes=['c2t'])
        act(sct, c2t, AF.Silu, reads=['c2t'], writes=['sct'])
        pv = bank(0, 32).rearrange("p (k r) -> p k r", r=2)
        transposes([(pv[:, k, :], sct[0:2, k * 128:(k + 1) * 128]) for k in range(KD)], ident_f[0:2, 0:2],
                   reads=['sct', 'ident_f'], writes=[PS(0)])
        cp('dve', sc2, pv, reads=[PS(0)], writes=['sc2'])
        for l in range(nlayers):
            dma('sync', brow, IN["b_ada"][l:l + 1, :].partition_broadcast(2), reads=['brow'], writes=['brow'])
            for g in range(24):
                wt = wa[g % 3]
                dma('pool', wt, IN["w_ada"][l][:, g * 512:(g + 1) * 512].rearrange("(k p) c -> p k c", p=128),
                    reads=[('wa', g % 3)], writes=[('wa', g % 3)])
                pb = 1 + g % 2
                mms(bank(pb, 512, 2), [(sc2[:, k, :], wt[:, k, :]) for k in range(KD)],
                    reads=['sc2', ('wa', g % 3)], writes=[PS(pb)])
                tt('dve', modrow[:, g * 512:(g + 1) * 512], bank(pb, 512, 2), brow[:, g * 512:(g + 1) * 512], ALU.add,
                   reads=[PS(pb), 'brow'], writes=['modrow'])
            dma('sync', GATE[l, :, 0, :], modrow[:, 2 * D:3 * D], reads=['modrow'], writes=[('GATE', l)])
            dma('sync', GATE[l, :, 1, :], modrow[:, 5 * D:6 * D], reads=['modrow'], writes=[('GATE', l)])
            pm = bank(3, 192).rearrange("p (j r) -> p j r", r=2)
            transposes([(pm[:, j, :], modrow[0:2, j * 128:(j + 1) * 128]) for j in range(96)], ident_f[0:2, 0:2],
                       reads=['modrow', 'ident_f'], writes=[PS(3)])
            cp('dve', modT[:, l, :, :], pm, reads=[PS(3)], writes=['modT'])
            for r in range(2):
                ts('dve', AB[:, l, r, 0, :], modT[:, l, 16:32, r], 1.0, None, ALU.add, None, reads=['modT'], writes=['AB'])
                tt('dve', AB[:, l, r, 0, :], AB[:, l, r, 0, :], featT[:, l, 0, F_N1G:F_N1G + 16], ALU.mult, reads=['AB', 'featT'], writes=['AB'])
                cp('dve', AB[:, l, r, 1, :], modT[:, l, 0:16, r], reads=['modT'], writes=['AB'])
                ts('dve', AB[:, l, r, 2, :], modT[:, l, 64:80, r], 1.0, None, ALU.add, None, reads=['modT'], writes=['AB'])
                tt('dve', AB[:, l, r, 2, :], AB[:, l, r, 2, :], featT[:, l, 0, F_N2G:F_N2G + 16], ALU.mult, reads=['AB', 'featT'], writes=['AB'])
                cp('dve', AB[:, l, r, 3, :], modT[:, l, 48:64, r], reads=['modT'], writes=['AB'])
            dma('sync', dl, IN["diff_lambda"][l:l + 1].partition_broadcast(128), reads=['dl'], writes=['dl'])
            tt('dve', pr, dl[:, 0:4:2, :], dl[:, 1:4:2, :], ALU.mult, reads=['dl'], writes=['pr'])
            S.add('dve', lambda e, sm=sm, pr=pr: e.tensor_reduce(out=sm, in_=pr, axis=AX.X, op=ALU.add), reads=['pr'], writes=['sm'])
            act(sm, sm, AF.Exp, reads=['sm'], writes=['sm'])
            lam_init = 0.8 - 0.6 * math.exp(-0.3 * l)
            tt('dve', lamv[:, l, 0:1], sm[:, 1:2], sm[:, 0:1], ALU.subtract, reads=['sm'], writes=['lamv'])
            ts('dve', lamv[:, l, 0:1], lamv[:, l, 0:1], -lam_init, None, ALU.add, None, reads=['lamv'], writes=['lamv'])
            act(lruS[:, l, 0, :], featT[:, l, 1, F_LAM - 128:F_LAM - 128 + 16], AF.Exp, reads=['featT'], writes=['lruS'], scale=-1.0)
            ts('dve', lruS[:, l, 0, :], lruS[:, l, 0, :], 1.0, None, ALU.add, None, reads=['lruS'], writes=['lruS'])
            act(lruS[:, l, 0, :], lruS[:, l, 0, :], AF.Ln, reads=['lruS'], writes=['lruS'])
            ts('dve', lruS[:, l, 1, :], lruS[:, l, 0, :], -16.0, None, ALU.mult, None, reads=['lruS'], writes=['lruS'])
            ts('dve', lruS[:, l, 0, :], lruS[:, l, 0, :], -8.0, None, ALU.mult, None, reads=['lruS'], writes=['lruS'])
            S.barrier()
        ar.release()
        S.barrier()

        chk('0')

        def hsrc(l, t):
            if l == 0:
                return IN["ctx"][t * 128:(t + 1) * 128, :] if t < 2 else IN["x"][(t - 2) * 128:(t - 1) * 128, :]
            return H[t * 128:(t + 1) * 128, :]

        def nm_A(hbuf, hb_tok, junk, ss, xn, i):
            act(junk, hbuf, AF.Square, reads=[hb_tok], writes=[('junk', i), ('ss', i)], accum=ss[:, 0:1])
            ts('dve', ss[:, 1:2], ss[:, 0:1], 1.0 / D, EPS, ALU.mult, ALU.add, reads=[('ss', i)], writes=[('ss', i)])
            act(ss[:, 2:3], ss[:, 1:2], AF.Ln, reads=[('ss', i)], writes=[('ss', i)])
            act(ss[:, 3:4], ss[:, 2:3], AF.Exp, reads=[('ss', i)], writes=[('ss', i)], scale=-0.5)
            act(xn, hbuf, AF.Identity, reads=[hb_tok, ('ss', i)], writes=[('xn', i)], scale=ss[:, 3:4])

        def nm_B(l, t, which, dst_fn, xn, pst, pst_tok, i):
            r = 1 if t < 2 else 0
            transposes([(pst[:, c, :], xn[:, c * 128:(c + 1) * 128]) for c in range(KD)], ident_b,
                       reads=[('xn', i), 'ident_b'], writes=pst_tok)
            for c in range(KD):
                d_ap, d_tok = dst_fn(c)
                ts('dve', d_ap, pst[:, c, :], AB[:, l, r, which, c:c + 1], AB[:, l, r, which + 1, c:c + 1], ALU.mult, ALU.add,
                   reads=pst_tok + ['AB'], writes=[d_tok])

        def pst_view(b0):
            return psum[:, b0 * 512:(b0 + 2) * 512].bitcast(BF16).rearrange("p (c t) -> p c t", t=128)

        for l in range(nlayers):
            last = (l == DEPTH - 1)
            lam_init = 0.8 - 0.6 * math.exp(-0.3 * l)
            blocks = BLKS
            ar.mark()
            uT = ar.alloc([KD, T], BF16)
            ar.mark()
            hb = [ar.alloc([D], F32) for _ in range(2)]
            junk = ar.alloc([D], BF16)
            xn = [ar.alloc([D], BF16) for _ in range(2)]
            ssb = [ar.alloc([4], F32) for _ in range(2)]
            def A1(t):
                i = t % 2
                dma('sync', hb[i], hsrc(l, t), reads=[('hb', i)], writes=[('hb', i)])
                nm_A(hb[i], ('hb', i), junk, ssb[i], xn[i], i)

            def B1(t):
                i = t % 2
                nm_B(l, t, 0, lambda c, t=t: (uT[:, c, t * 128:(t + 1) * 128], ('uT', t)),
                     xn[i], pst_view(2 * i), [PS(2 * i), PS(2 * i + 1)], i)
            A1(0)
            for t in range(NT):
                if t + 1 < NT:
                    A1(t + 1)
                B1(t)
            S.barrier()
            ar.release()
            chk('1a')
            wg = [ar.alloc([KD, 512], BF16) for _ in range(3)]
            cosT = ar.alloc([T], F32)
            sinT = ar.alloc([T], F32)
            stage_b = [ar.alloc([4, 512], BF16) for _ in range(2)]
            stage_f = [ar.alloc([4, 512], F32) for _ in range(2)]
            stage_v = [ar.alloc([512], BF16) for _ in range(2)]
            sqb = [ar.alloc([512], BF16) for _ in range(3)]
            xgb = [ar.alloc([512], BF16) for _ in range(3)]
            sdt = [ar.alloc([512], F32) for _ in range(3)]
            t1t = [ar.alloc([512], F32) for _ in range(3)]
            t2t = [ar.alloc([512], F32) for _ in range(3)]
            pend = []
            dma('sync', cosT, IN["cosT"], writes=['cosT'])
            dma('sync', sinT, IN["sinT"], writes=['sinT'])
            cnt = {'pb': 0, 'q': 0, 'st': 0, 'sv': 0}
            ut_all = [('uT', t) for t in range(NT)]

            import os as _os
            def qpath(P0, pb, n, t0, gq, dst, dtok):
                qi = cnt['q'] % 3
                pi_ = cnt['q'] % 2
                cnt['q'] += 1
                act(t2t[qi][:, 0:n], P0, AF.Copy, reads=[PS(pb)], writes=[('t2t', qi)])
                act(sqb[qi][:, 0:n], t2t[qi][:, 0:n], AF.Square, reads=[('t2t', qi)], writes=[('sqb', qi)])
                ts('dve', xgb[qi][:, 0:n], t2t[qi][:, 0:n], gq, None, ALU.mult, None, reads=[('t2t', qi), 'featT'], writes=[('xgb', qi)])

                def post():
                    p1, p2 = 3 + pi_, 5 + pi_
                    mms(bank(p1, n), [(ones_b, sqb[qi][:, 0:n])], reads=['ones_b', ('sqb', qi)], writes=[PS(p1)])
                    mms(bank(p2, n), [(rotm, xgb[qi][:, 0:n])], reads=['rotm', ('xgb', qi)], writes=[PS(p2)])
                    act(sdt[qi][:, 0:n], bank(p1, n), AF.Ln, reads=[PS(p1), 'epsc'], writes=[('sdt', qi)], bias=eps_ap, scale=1.0 / HD)
                    act(sdt[qi][:, 0:n], sdt[qi][:, 0:n], AF.Exp, reads=[('sdt', qi)], writes=[('sdt', qi)], scale=-0.5)
                    tt('pool', t1t[qi][:, 0:n], xgb[qi][:, 0:n], cosT[:, t0:t0 + n], ALU.mult, reads=[('xgb', qi), 'cosT'], writes=[('t1t', qi)])
                    tt('dve', t2t[qi][:, 0:n], bank(p2, n), sinT[:, t0:t0 + n], ALU.mult, reads=[PS(p2), 'sinT'], writes=[('t2t', qi)])
                    tt('pool', t1t[qi][:, 0:n], t1t[qi][:, 0:n], t2t[qi][:, 0:n], ALU.add, reads=[('t1t', qi), ('t2t', qi)], writes=[('t1t', qi)])
                    tt('dve', dst, t1t[qi][:, 0:n], sdt[qi][:, 0:n], ALU.mult, reads=[('t1t', qi), ('sdt', qi)], writes=[dtok])
                return post

            def flush():
                while pend:
                    pend.pop(0)()

            _gl = [int(a) for a in _os.environ['DBG_GROUPS'].split(',')] if 'DBG_GROUPS' in _os.environ else list(range(25))
            for g in _gl:
                if g < 2:
                    kind, dstT, base, gq = 'q', QA, g * 4, FT(l, F_QNA)
                elif g < 4:
                    kind, dstT, base, gq = 'q', KA, (g - 2) * 4, FT(l, F_KNA)
                elif g < 6:
                    kind = 'v'
                elif g < 8:
                    kind, dstT, base = 'bx', BX, (g - 6) * 4
                elif g < 10:
                    kind, dstT, base = 'by', BYG, (g - 8) * 4
                elif g < 12:
                    kind, dstT, base, gq = 'q', QC, (g - 10) * 4, FT(l, F_QNC)
                elif g == 12:
                    kind = 'ckv'
                else:
                    kind, dstT, base = 'gate', G, (g - 13) * 4
                need_ctx = (not last) or kind in ('v', 'bx', 'ckv') or (kind == 'q' and dstT is KA)
                wi = g % 3
                dma('pool', wg[wi], IN["w_in"][l][:, g * 512:(g + 1) * 512].rearrange("(k p) c -> p k c", p=128),
                    reads=[('wg', wi)], writes=[('wg', wi)])
                for (t0, n) in blocks:
                    if t0 == 0 and not need_ctx:
                        continue
                    if kind in ('v', 'ckv'):
                        c0, nc_ = (0, 512) if kind == 'v' else (256, 256)
                        for tt_ in range(t0 // 128, (t0 + n) // 128):
                            pb = cnt['pb'] % 3
                            cnt['pb'] += 1
                            mms(bank(pb, nc_), [(uT[:, k, tt_ * 128:(tt_ + 1) * 128], wg[wi][:, k, c0:c0 + nc_]) for k in range(KD)],
                                reads=[('uT', tt_), ('wg', wi)], writes=[PS(pb)])
                            si = cnt['sv'] % 2
                            cnt['sv'] += 1
                            act(stage_v[si][:, 0:nc_], bank(pb, nc_), AF.Copy, reads=[PS(pb)], writes=[('sv', si)])
                            if kind == 'v':
                                dma('sync', VA[tt_ * 128:(tt_ + 1) * 128, (g - 4) * 512:(g - 3) * 512], stage_v[si], reads=[('sv', si)], writes=[U('VA')])
                            else:
                                dma('sync', VC[tt_ * 128:(tt_ + 1) * 128, :], stage_v[si][:, 0:256], reads=[('sv', si)], writes=[U('VC')])
                    ncc = 2 if kind == 'ckv' else (0 if kind == 'v' else 4)
                    if ncc == 0:
                        continue
                    si = cnt['st'] % 2
                    cnt['st'] += 1
                    utk = [('uT', t_) for t_ in range(t0 // 128, (t0 + n) // 128)]
                    for cc in range(ncc):
                        pb = cnt['pb'] % 3
                        cnt['pb'] += 1
                        P0 = bank(pb, n)
                        mms(P0, [(wg[wi][:, k, cc * 128:(cc + 1) * 128], uT[:, k, t0:t0 + n]) for k in range(KD)],
                            reads=utk + [('wg', wi)], writes=[PS(pb)])
                        if kind == 'bx':
                            act(stage_f[si][:, cc, 0:n], P0, AF.Copy, reads=[PS(pb)], writes=[('stf', si)])
                        elif kind == 'by':
                            act(stage_b[si][:, cc, 0:n], P0, AF.Gelu_apprx_tanh, reads=[PS(pb)], writes=[('stb', si)])
                        elif kind == 'gate':
                            act(stage_b[si][:, cc, 0:n], P0, AF.Sigmoid, reads=[PS(pb), 'featT'], writes=[('stb', si)],
                                bias=FT(l, F_BG + base + cc))
                        elif kind == 'q':
                            post = qpath(P0, pb, n, t0, gq, stage_b[si][:, cc, 0:n], ('stb', si))
                            flush()
                            pend.append(post)
                        elif kind == 'ckv':
                            post = qpath(P0, pb, n, t0, FT(l, F_KNC), stage_b[si][:, cc, 0:n], ('stb', si))
                            flush()
                            pend.append(post)
                        if kind not in ('q', 'ckv'):
                            flush()
                    if kind == 'bx':
                        dma('sync', BX[base:base + 4, :, t0:t0 + n].rearrange("c p t -> p c t"), stage_f[si][:, :, 0:n],
                            reads=[('stf', si)], writes=[U('BX')])
                    elif kind == 'ckv':
                        pend.append(lambda si=si, t0=t0, n=n: dma('sync', KC[:, :, t0:t0 + n].rearrange("c p t -> p c t"), stage_b[si][:, 0:2, 0:n],
                                                                 reads=[('stb', si)], writes=[U('KC')]))
                    elif kind == 'q':
                        pend.append(lambda si=si, t0=t0, n=n, dstT=dstT, base=base: dma('sync', dstT[base:base + 4, :, t0:t0 + n].rearrange("c p t -> p c t"),
                                                                                      stage_b[si][:, :, 0:n], reads=[('stb', si)], writes=[U('dst')]))
                    else:
                        dma('sync', dstT[base:base + 4, :, t0:t0 + n].rearrange("c p t -> p c t"), stage_b[si][:, :, 0:n],
                            reads=[('stb', si)], writes=[U('dst')])
            flush()
            S.barrier()
            ar.release()
            if phases is not None and 2 not in phases:
                continue

            ar.mark()
            KAs = ar.alloc([8, T], BF16)
            VAs = ar.alloc([NT, 1024], BF16)
            KCs = ar.alloc([2, T], BF16)
            VCs = ar.alloc([NT, 256], BF16)
            qb_ = [ar.alloc([512], BF16) for _ in range(2)]
            ptile = [ar.alloc([2, 512], BF16) for _ in range(3)]
            rden = ar.alloc([512], F32)
            lnd = ar.alloc([512], F32)
            oraw = [ar.alloc([3, 512], F32) for _ in range(2)]
            tailq = []
            tailq2 = []
            on_ = ar.alloc([2, 2, 512], F32)
            ot = ar.alloc([2, 512], F32)
            sq2 = ar.alloc([2, 512], BF16)
            rs_ = ar.alloc([512], F32)
            sga = ar.alloc([2], F32)
            yst = [ar.alloc([2, 512], BF16) for _ in range(2)]
            dma('sync', KAs, KA.rearrange("h p t -> p h t"), writes=['KAs'])
            dma('sync', VAs, VA.rearrange("(k p) c -> p k c", p=128), writes=['VAs'])
            dma('sync', KCs, KC.rearrange("h p t -> p h t"), writes=['KCs'])
            dma('sync', VCs, VC.rearrange("(k p) c -> p k c", p=128), writes=['VCs'])
            ts('dve', sga, featT[:, l, 1, F_SUB - 128:F_SUB - 128 + 2], 1.0 - lam_init, None, ALU.mult, None, reads=['featT'], writes=['sga'])
            sc_ = float(HD) ** -0.5
            cnt2 = {'q': 0, 'p': 0, 'y': 0, 'o': 0}
            qblocks = [(256, 512, 0, NT), (768, 512, 0, NT), (1280, 512, 0, NT), (1792, 512, 0, NT)]
            if not last:
                qblocks = [(0, 256, 0, 2)] + qblocks

            def attn_map(Qsrc, Ks, Vlist, t0, n, k0, k1):
                qi = cnt2['q'] % 2
                cnt2['q'] += 1
                dma('sync', qb_[qi][:, 0:n], Qsrc[:, t0:t0 + n], reads=[('qb', qi)], writes=[('qb', qi)])
                kts = list(range(k0, k1))

                assert len(kts) % 2 == 0
                nst = len(kts) // 2
                PAIRS = [(0, 1), (5, 6)]

                def pair_view(p0):
                    return psum[:, p0 * 512:(p0 + 2) * 512].rearrange("p (b c) -> p b c", c=512)[:, :, 0:n]

                def score(s_):
                    p0, p1 = PAIRS[s_ % 2]
                    ka, kb = kts[2 * s_], kts[2 * s_ + 1]

                    def f(e):
                        e.matmul(bank(p0, n), Ks[:, ka * 128:(ka + 1) * 128], qb_[qi][:, 0:n], start=True, stop=True)
                        return e.matmul(bank(p1, n), Ks[:, kb * 128:(kb + 1) * 128], qb_[qi][:, 0:n], start=True, stop=True)
                    S.add('pe', f, reads=['K', ('qb', qi)], writes=[PS(p0), PS(p1)])
                score(0)
                if nst > 1:
                    score(1)
                for s_ in range(nst):
                    p0, p1 = PAIRS[s_ % 2]
                    pi = cnt2['p'] % 3
                    cnt2['p'] += 1
                    act(ptile[pi][:, :, 0:n], pair_view(p0), AF.Exp, reads=[PS(p0), PS(p1)], writes=[('pt', pi)], scale=sc_)
                    if s_ == 0:
                        while tailq:
                            tailq.pop(0)()
                    if s_ == min(3, nst - 1):
                        while tailq2:
                            tailq2.pop(0)()

                    def pvf(e, s_=s_, pi=pi):
                        ins = None
                        for h_ in range(2):
                            j = 2 * s_ + h_
                            kt = kts[j]
                            for (vf, b_) in Vlist:
                                ins = e.matmul(bank(b_, n), vf(kt), ptile[pi][:, h_, 0:n], start=(j == 0), stop=(j == len(kts) - 1))
                            ins = e.matmul(bank(4, n), ones_b, ptile[pi][:, h_, 0:n], start=(j == 0), stop=(j == len(kts) - 1))
                        return ins
                    S.add('pe', pvf, reads=[('pt', pi), 'V', 'ones_b'], writes=[PS(b_) for (_, b_) in Vlist] + [PS(4)])
                    if s_ + 2 < nst:
                        score(s_ + 2)
                oi = cnt2['o'] % 2
                cnt2['o'] += 1
                for ci, (_, b_) in enumerate(Vlist):
                    cp('dve', oraw[oi][:, ci, 0:n], bank(b_, n), reads=[PS(b_)], writes=[('oraw', oi)])
                cp('dve', oraw[oi][:, 2, 0:n], bank(4, n), reads=[PS(4)], writes=[('oraw', oi)])

                def norm():
                    act(lnd[:, 0:n], oraw[oi][:, 2, 0:n], AF.Ln, reads=[('oraw', oi)], writes=['lnd'])
                    act(rden[:, 0:n], lnd[:, 0:n], AF.Exp, reads=['lnd'], writes=['rden'], scale=-1.0)
                tailq.append(norm)
                return oi

            for (t0, n, k0, k1) in qblocks:
                for h in range(4):
                    for j in range(2):
                        i = 2 * h + j
                        oi = attn_map(QA[i], KAs[:, i, :], [(lambda kt, h=h: VAs[:, kt, h * 256:h * 256 + 128], 2),
                                                             (lambda kt, h=h: VAs[:, kt, h * 256 + 128:h * 256 + 256], 3)], t0, n, k0, k1)
                        def normA(j=j, oi=oi, n=n):
                            for c in range(2):
                                tt('dve', on_[:, j, c, 0:n], oraw[oi][:, c, 0:n], rden[:, 0:n], ALU.mult, reads=[('oraw', oi), 'rden'], writes=[('on', j)])
                        tailq.append(normA)

                    def headA(h=h, n=n, t0=t0):
                        for c in range(2):
                            stt(ot[:, c, 0:n], on_[:, 1, c, 0:n], lamv[:, l, 0:1], on_[:, 0, c, 0:n], ALU.mult, ALU.add,
                                reads=[('on', 0), ('on', 1), 'lamv'], writes=['ot'])
                        tt('pool', sq2[:, :, 0:n], ot[:, :, 0:n], ot[:, :, 0:n], ALU.mult, reads=['ot'], writes=['sq2'])
                        tailq2.append(lambda: headA2(h, n, t0))

                    def headA2(h, n, t0):
                        mms(bank(7, n), [(ones_b, sq2[:, 0, 0:n]), (ones_b, sq2[:, 1, 0:n])], reads=['sq2', 'ones_b'], writes=[PS(7)])
                        act(rs_[:, 0:n], bank(7, n), AF.Ln, reads=[PS(7), 'epsc'], writes=['rs'], bias=eps_ap, scale=1.0 / 256)
                        act(rs_[:, 0:n], rs_[:, 0:n], AF.Exp, reads=['rs'], writes=['rs'], scale=-0.5)
                        yi = cnt2['y'] % 2
                        cnt2['y'] += 1
                        for c in range(2):
                            stt(yst[yi][:, c, 0:n], ot[:, c, 0:n], sga[:, c:c + 1], rs_[:, 0:n], ALU.mult, ALU.mult,
                                reads=['ot', 'sga', 'rs'], writes=[('yst', yi)])
                        dma('sync', YA[2 * h:2 * h + 2, :, t0:t0 + n].rearrange("c p t -> p c t"), yst[yi][:, :, 0:n],
                            reads=[('yst', yi)], writes=[U('YA')])
                    tailq.append(headA)
                for hq in range(8):
                    kv = hq // 4
                    oi = attn_map(QC[hq], KCs[:, kv, :], [(lambda kt, kv=kv: VCs[:, kt, kv * 128:(kv + 1) * 128], 2)], t0, n, k0, k1)
                    def normC(hq=hq, oi=oi, n=n, t0=t0):
                        yi = cnt2['y'] % 2
                        cnt2['y'] += 1
                        tt('dve', yst[yi][:, 0, 0:n], oraw[oi][:, 0, 0:n], rden[:, 0:n], ALU.mult, reads=[('oraw', oi), 'rden'], writes=[('yst', yi)])
                        dma('sync', YC[hq, :, t0:t0 + n], yst[yi][:, 0, 0:n], reads=[('yst', yi)], writes=[U('YC')])
                    tailq.append(normC)
            while tailq:
                tailq.pop(0)()
            while tailq2:
                tailq2.pop(0)()
            S.barrier()
            ar.release()
            if phases is not None and 3 not in phases:
                continue

            ar.mark()
            lw = ar.alloc([2, 2, 8, 128], BF16)
            bxt = ar.alloc([T], F32)
            xc = ar.alloc([T], F32)
            xcb = ar.alloc([T], BF16)
            rr = ar.alloc([T], F32)
            ii = ar.alloc([T], F32)
            aa = ar.alloc([T], F32)
            bb = ar.alloc([T], F32)
            hh = [ar.alloc([T], F32) for _ in range(2)]
            byg = ar.alloc([T], BF16)
            ybo = ar.alloc([T], BF16)
            dma('pool', lw[:, 0], IN["lru_w_a"][l].rearrange("d n c e -> c d n e"), writes=['lw'])
            dma('pool', lw[:, 1], IN["lru_w_x"][l].rearrange("d n c e -> c d n e"), writes=['lw'])
            segs = [(0, TC), (TC, TL)]
            for n_ in range(8):
                dma('sync', bxt, BX[n_], reads=['bxt'], writes=['bxt'])
                dma('sync', byg, BYG[n_], reads=['byg'], writes=['byg'])
                cw = [FT(l, F_CW + k * 8 + n_) for k in range(4)]
                cb = FT(l, F_CB + n_)
                for (s0, sn) in segs:
                    ts('dve', xc[:, s0:s0 + sn], bxt[:, s0:s0 + sn], cw[2], cb, ALU.mult, ALU.add, reads=['bxt', 'featT'], writes=['xc'])
                    stt(xc[:, s0 + 2:s0 + sn], bxt[:, s0:s0 + sn - 2], cw[0], xc[:, s0 + 2:s0 + sn], ALU.mult, ALU.add, reads=['bxt', 'xc'], writes=['xc'])
                    stt(xc[:, s0 + 1:s0 + sn], bxt[:, s0:s0 + sn - 1], cw[1], xc[:, s0 + 1:s0 + sn], ALU.mult, ALU.add, reads=['bxt', 'xc'], writes=['xc'])
                    stt(xc[:, s0:s0 + sn - 1], bxt[:, s0 + 1:s0 + sn], cw[3], xc[:, s0:s0 + sn - 1], ALU.mult, ALU.add, reads=['bxt', 'xc'], writes=['xc'])
                cp('pool', xcb, xc, reads=['xc'], writes=['xcb'])
                for d_ in range(2):
                    ba = FT(l, F_BA + d_ * 8 + n_)
                    bx_ = FT(l, F_BX + d_ * 8 + n_)
                    s8 = lruS[:, l, 0, d_ * 8 + n_:d_ * 8 + n_ + 1]
                    s16 = lruS[:, l, 1, d_ * 8 + n_:d_ * 8 + n_ + 1]
                    for bi, (t0, n) in enumerate(BLKS):
                        pa, px = (bi % 2) * 2, (bi % 2) * 2 + 1
                        mms(bank(pa, n), [(lw[:, 0, d_, n_, :], xcb[:, t0:t0 + n])], reads=['lw', 'xcb'], writes=[PS(pa)])
                        mms(bank(px, n), [(lw[:, 1, d_, n_, :], xcb[:, t0:t0 + n])], reads=['lw', 'xcb'], writes=[PS(px)])
                        act(rr[:, t0:t0 + n], bank(pa, n), AF.Sigmoid, reads=[PS(pa), 'featT'], writes=['rr'], bias=ba)
                        act(ii[:, t0:t0 + n], bank(px, n), AF.Sigmoid, reads=[PS(px), 'featT'], writes=['ii'], bias=bx_)
                    act(aa, rr, AF.Exp, reads=['rr', 'lruS'], writes=['aa'], scale=s8)
                    act(bb, rr, AF.Exp, reads=['rr', 'lruS'], writes=['bb'], scale=s16)
                    ts('pool', bb, bb, -1.0, 1.0, ALU.mult, ALU.add, reads=['bb'], writes=['bb'])
                    act(bb, bb, AF.Sqrt, reads=['bb'], writes=['bb'])
                    tt('dve', bb, bb, ii, ALU.mult, reads=['bb', 'ii'], writes=['bb'])
                    tt('dve', bb, bb, xc, ALU.mult, reads=['bb', 'xc'], writes=['bb'])
                    hd = hh[d_]
                    if d_ == 0:
                        S.add('dve', lambda e, hd=hd: e.tensor_tensor_scan(out=hd, data0=aa, data1=bb, initial=0.0, op0=ALU.mult, op1=ALU.add),
                              reads=['aa', 'bb'], writes=[('hh', d_)])
                    else:
                        S.add('dve', lambda e, hd=hd: e.tensor_tensor_scan(out=hd[:, TC - 1::-1] if False else hd[:, 0:TC][:, ::-1], data0=aa[:, 0:TC][:, ::-1],
                                                                            data1=bb[:, 0:TC][:, ::-1], initial=0.0, op0=ALU.mult, op1=ALU.add),
                              reads=['aa', 'bb'], writes=[('hh', d_)])
                        S.add('dve', lambda e, hd=hd: e.tensor_tensor_scan(out=hd[:, TC:T][:, ::-1], data0=aa[:, TC:T][:, ::-1],
                                                                            data1=bb[:, TC:T][:, ::-1], initial=hd[:, 0:1], op0=ALU.mult, op1=ALU.add),
                              reads=['aa', 'bb', ('hh', d_)], writes=[('hh', d_)])
                tt('pool', hh[0], hh[0], hh[1], ALU.add, reads=[('hh', 0), ('hh', 1)], writes=[('hh', 0)])
                tt('dve', ybo, hh[0], byg, ALU.mult, reads=[('hh', 0), 'byg'], writes=['ybo'])
                dma('sync', YB[n_], ybo, reads=['ybo'], writes=[U('YB')])
            S.barrier()
            ar.release()
            if phases is not None and 4 not in phases:
                continue

            ar.mark()
            wbr = ar.alloc([3, 8, D], BF16)
            yT = [ar.alloc([3, 8, 512], BF16) for _ in range(2)]
            gt = [ar.alloc([3, 512], BF16) for _ in range(2)]
            mst = [ar.alloc([KD, 512], BF16) for _ in range(2)]
            mtmp = [ar.alloc([2, 512], F32) for _ in range(2)]
            for bi_, nm in enumerate(["w_branch_a", "w_branch_b", "w_branch_c"]):
                dma('pool', wbr[:, bi_], IN[nm][l].rearrange("(k p) c -> p k c", p=128), writes=['wbr'])
            mblocks = BLKS if not last else BLKS[1:]
            for bi, (t0, n) in enumerate(mblocks):
                yi = bi % 2
                for bi_, Ysrc in enumerate([YA, YB, YC]):
                    dma('sync', yT[yi][:, bi_, :, 0:n], Ysrc[:, :, t0:t0 + n].rearrange("c p t -> p c t"), reads=[('yT', yi)], writes=[('yT', yi)])
                for dc in range(KD):
                    gi = dc % 2
                    dma('sync', gt[gi][:, :, 0:n], G[dc:48:16, :, t0:t0 + n].rearrange("c p t -> p c t"), reads=[('gt', gi)], writes=[('gt', gi)])
                    pbs = [(dc % 2) * 3 + b_ for b_ in range(3)]
                    for b_ in range(3):
                        mms(bank(pbs[b_], n), [(wbr[:, b_, k, dc * 128:(dc + 1) * 128], yT[yi][:, b_, k, 0:n]) for k in range(8)],
                            reads=['wbr', ('yT', yi)], writes=[PS(pbs[b_])])
                    tm = mtmp[gi]
                    tt('dve', tm[:, 0, 0:n], bank(pbs[0], n), gt[gi][:, 0, 0:n], ALU.mult, reads=[PS(pbs[0]), ('gt', gi)], writes=[('tm0', gi)])
                    tt('dve', tm[:, 1, 0:n], bank(pbs[1], n), gt[gi][:, 1, 0:n], ALU.mult, reads=[PS(pbs[1]), ('gt', gi)], writes=[('tm1', gi)])
                    tt('pool', tm[:, 0, 0:n], tm[:, 0, 0:n], tm[:, 1, 0:n], ALU.add, reads=[('tm0', gi), ('tm1', gi)], writes=[('tm0', gi)])
                    tt('dve', tm[:, 1, 0:n], bank(pbs[2], n), gt[gi][:, 2, 0:n], ALU.mult, reads=[PS(pbs[2]), ('gt', gi)], writes=[('tm1', gi)])
                    tt('pool', mst[yi][:, dc, 0:n], tm[:, 0, 0:n], tm[:, 1, 0:n], ALU.add, reads=[('tm0', gi), ('tm1', gi)], writes=[('mst', yi)])
                dma('sync', MT[:, :, t0:t0 + n].rearrange("c p t -> p c t"), mst[yi][:, :, 0:n], reads=[('mst', yi)], writes=[U('MT')])
            S.barrier()
            ar.release()

            ar.mark()
            wout = ar.alloc([KD, D], BF16)
            wr = ar.alloc([KD, 36], BF16)
            g1b = ar.alloc([2, D], F32)
            rbias = ar.alloc([36], F32)
            mTt = [ar.alloc([KD, 128], BF16) for _ in range(2)]
            hb2 = [ar.alloc([D], F32) for _ in range(2)]
            ytmp = ar.alloc([D], F32)
            junk = ar.alloc([D], BF16)
            xn2 = [ar.alloc([D], BF16) for _ in range(2)]
            ss2 = [ar.alloc([4], F32) for _ in range(2)]
            vTt = [ar.alloc([KD, 128], BF16) for _ in range(2)]
            rt = ar.alloc([96], F32)
            cmb = ar.alloc([NEXP], F32)
            cmbT = [ar.alloc([128], F32) for _ in range(2)]
            dma('pool', wout, IN["w_out"][l].rearrange("(k p) c -> p k c", p=128), writes=['wout'])
            dma('pool', wr[:, :, 0:4], IN["w_group"][l].rearrange("(k p) c -> p k c", p=128), writes=['wr'])
            dma('pool', wr[:, :, 4:36], IN["w_route"][l].rearrange("(k p) c -> p k c", p=128), writes=['wr'])
            dma('sync', g1b[:, 0, :], GATE[l, 0:1, 0, :].partition_broadcast(128), writes=['g1b'])
            dma('sync', g1b[:, 1, :], GATE[l, 1:2, 0, :].partition_broadcast(128), writes=['g1b'])
            dma('sync', rbias[:, 0:4], IN["b_group"][l:l + 1, :].partition_broadcast(128), writes=['rbias'])
            dma('sync', rbias[:, 4:36], IN["b_route"][l:l + 1, :].partition_broadcast(128), writes=['rbias'])
            tiles = list(range(NT)) if not last else list(range(2, NT))
            def A4(ti):
                t = tiles[ti]
                i = ti % 2
                r = 1 if t < 2 else 0
                dma('sync', mTt[i], MT[:, :, t * 128:(t + 1) * 128].rearrange("c p t -> p c t"), reads=[('mTt', i)], writes=[('mTt', i)])
                dma('sync', hb2[i], hsrc(l, t), reads=[('hb2', i)], writes=[('hb2', i)])
                for cb_ in range(4):
                    mms(bank(cb_), [(mTt[i][:, k, :], wout[:, k, cb_ * 512:(cb_ + 1) * 512]) for k in range(KD)],
                        reads=[('mTt', i), 'wout'], writes=[PS(cb_)])
                    tt('dve', ytmp[:, cb_ * 512:(cb_ + 1) * 512], bank(cb_), g1b[:, r, cb_ * 512:(cb_ + 1) * 512], ALU.mult,
                       reads=[PS(cb_), 'g1b'], writes=['ytmp'])
                tt('pool', hb2[i], hb2[i], ytmp, ALU.add, reads=[('hb2', i), 'ytmp'], writes=[('hb2', i)])
                dma('sync', H[t * 128:(t + 1) * 128, :], hb2[i], reads=[('hb2', i)], writes=[('H', t)])
                nm_A(hb2[i], ('hb2', i), junk, ss2[i], xn2[i], ('n2', i))

            def B4(ti):
                t = tiles[ti]
                i = ti % 2
                nm_B(l, t, 2, lambda c, i=i: (vTt[i][:, c, :], ('vTt', i)), xn2[i], pst_view(4), [PS(4), PS(5)], ('n2', i))
                dma('sync', VT[:, :, t * 128:(t + 1) * 128].rearrange("c p t -> p c t"), vTt[i], reads=[('vTt', i)], writes=[U('VT')])
                router(ti)

            def router(ti):
                t = tiles[ti]
                i = ti % 2
                mms(bank(6, 36), [(vTt[i][:, k, :], wr[:, k, :]) for k in range(KD)], reads=[('vTt', i), 'wr'], writes=[PS(6)])
                lg = rt[:, 0:36]
                tt('dve', lg, bank(6, 36), rbias, ALU.add, reads=[PS(6), 'rbias'], writes=['rt'])
                gl_, el_ = rt[:, 0:4], rt[:, 4:36].rearrange("p (g e) -> p g e", e=8)
                gmax, oneh, gex, gsum = rt[:, 36:37], rt[:, 40:44], rt[:, 44:48], rt[:, 48:49]
                els, m8, sel, dif = rt[:, 52:60], rt[:, 60:68], rt[:, 68:76], rt[:, 76:78]
                R_ = ['rt']
                S.add('dve', lambda e, gmax=gmax, gl_=gl_: e.tensor_reduce(out=gmax, in_=gl_, axis=AX.X, op=ALU.max), reads=R_, writes=R_)
                ts('dve', oneh, gl_, gmax, None, ALU.is_equal, None, reads=R_, writes=R_)
                ts('dve', gex, gl_, gmax, None, ALU.subtract, None, reads=R_, writes=R_)
                act(gex, gex, AF.Exp, reads=R_, writes=R_)
                S.add('dve', lambda e, gsum=gsum, gex=gex: e.tensor_reduce(out=gsum, in_=gex, axis=AX.X, op=ALU.add), reads=R_, writes=R_)
                recip(gsum, gsum, reads=R_, writes=R_)
                ts('dve', els, el_[:, 0, :], oneh[:, 0:1], None, ALU.mult, None, reads=R_, writes=R_)
                for g_ in range(1, 4):
                    stt(els, el_[:, g_, :], oneh[:, g_:g_ + 1], els, ALU.mult, ALU.add, reads=R_, writes=R_)
                S.add('dve', lambda e, m8=m8, els=els: e.max(out=m8, in_=els), reads=R_, writes=R_)
                tt('dve', dif[:, 1:2], m8[:, 1:2], m8[:, 0:1], ALU.subtract, reads=R_, writes=R_)
                act(dif[:, 1:2], dif[:, 1:2], AF.Exp, reads=R_, writes=R_)
                ts('dve', dif[:, 0:1], dif[:, 1:2], 1.0, None, ALU.add, None, reads=R_, writes=R_)
                recip(dif[:, 0:1], dif[:, 0:1], reads=R_, writes=R_)
                tt('dve', dif[:, 1:2], dif[:, 1:2], dif[:, 0:1], ALU.mult, reads=R_, writes=R_)
                ts('dve', dif, dif, gsum, None, ALU.mult, None, reads=R_, writes=R_)
                ts('dve', sel, els, m8[:, 0:1], dif[:, 0:1], ALU.is_equal, ALU.mult, reads=R_, writes=R_)
                ts('dve', m8[:, 2:8] if False else rt[:, 80:88], els, m8[:, 1:2], dif[:, 1:2], ALU.is_equal, ALU.mult, reads=R_, writes=R_)
                tt('dve', sel, sel, rt[:, 80:88], ALU.add, reads=R_, writes=R_)
                cm = cmb.rearrange("p (g e) -> p g e", e=8)
                for g_ in range(4):
                    ts('dve', cm[:, g_, :], sel, oneh[:, g_:g_ + 1], None, ALU.mult, None, reads=R_, writes=['cmb'])
                transposes([(bank(7, 128, 32), cmb)], ident_f, reads=['cmb', 'ident_f'], writes=[PS(7)])
                cp('dve', cmbT[i][0:32, :], bank(7, 128, 32), reads=[PS(7)], writes=[('cmbT', i)])
                dma('sync', CMB[:, t * 128:(t + 1) * 128], cmbT[i][0:32, :], reads=[('cmbT', i)], writes=[U('CMB')])

            A4(0)
            for ti in range(len(tiles)):
                if ti + 1 < len(tiles):
                    A4(ti + 1)
                B4(ti)
            S.barrier()
            ar.release()
            if phases is not None and 5 not in phases:
                continue

            mb = [(0, 768), (768, 768), (1536, 768)] if not last else [(256, 768), (1024, 768), (1792, 512)]
            for (b0, bn) in mb:
                ar.mark()
                ntt = bn // 128
                nsb = 2 if bn > 512 else 1
                sbn = bn // nsb
                vTb = ar.alloc([KD, bn], BF16)
                acc = ar.alloc([ntt, D], F32)
                w1t = [ar.alloc([KD, DEXP], BF16) for _ in range(2)]
                w3t = [ar.alloc([KD, DEXP], BF16) for _ in range(2)]
                w2t = ar.alloc([4, D], BF16)
                ar.mark()
                hT = [ar.alloc([4, bn], BF16) for _ in range(2)]
                cbt = [ar.alloc([bn], F32) for _ in range(2)]
                s1 = [ar.alloc([sbn], F32) for _ in range(2)]
                dma('sync', vTb, VT[:, :, b0:b0 + bn].rearrange("c p t -> p c t"), writes=['vTb'])
                S.add('pool', lambda e, acc=acc: e.memset(acc, 0.0), writes=['acc'])
                k5 = {'p': 0, 'o': 0}
                for ex in range(NEXP):
                    wi = ex % 2
                    dma('pool', w1t[wi], IN["w1"][l, ex].rearrange("(k p) c -> p k c", p=128), reads=[('w1', wi)], writes=[('w1', wi)])
                    dma('pool', w3t[wi], IN["w3"][l, ex].rearrange("(k p) c -> p k c", p=128), reads=[('w3', wi)], writes=[('w3', wi)])
                    dma('pool', w2t, IN["w2"][l, ex].rearrange("(k p) c -> p k c", p=128), reads=['w2'], writes=['w2'])
                    dma('sync', cbt[wi], CMB[ex:ex + 1, b0:b0 + bn].partition_broadcast(128), reads=[('cbt', wi)], writes=[('cbt', wi)])
                    for sb_ in range(nsb):
                        q0 = sb_ * sbn
                        for dc in range(4):
                            pi = k5['p'] % 2
                            k5['p'] += 1
                            p1, p3 = pi * 2, pi * 2 + 1
                            mms(bank(p1, sbn), [(w1t[wi][:, k, dc * 128:(dc + 1) * 128], vTb[:, k, q0:q0 + sbn]) for k in range(KD)],
                                reads=[('w1', wi), 'vTb'], writes=[PS(p1)])
                            mms(bank(p3, sbn), [(w3t[wi][:, k, dc * 128:(dc + 1) * 128], vTb[:, k, q0:q0 + sbn]) for k in range(KD)],
                                reads=[('w3', wi), 'vTb'], writes=[PS(p3)])
                            act(s1[pi], bank(p1, sbn), AF.Silu, reads=[PS(p1)], writes=[('s1', pi)])
                            tt('dve', s1[pi], s1[pi], bank(p3, sbn), ALU.mult, reads=[('s1', pi), PS(p3)], writes=[('s1', pi)])
                            tt('dve', hT[wi][:, dc, q0:q0 + sbn], s1[pi], cbt[wi][:, q0:q0 + sbn], ALU.mult,
                               reads=[('s1', pi), ('cbt', wi)], writes=[('hT', wi)])
                    for tt_ in range(ntt):
                        for cb_ in range(4):
                            po = 4 + k5['o'] % 4
                            k5['o'] += 1
                            mms(bank(po), [(hT[wi][:, dc, tt_ * 128:(tt_ + 1) * 128], w2t[:, dc, cb_ * 512:(cb_ + 1) * 512]) for dc in range(4)],
                                reads=[('hT', wi), 'w2'], writes=[PS(po)])
                            tt('dve', acc[:, tt_, cb_ * 512:(cb_ + 1) * 512], acc[:, tt_, cb_ * 512:(cb_ + 1) * 512], bank(po), ALU.add,
                               reads=[PS(po), ('acc', tt_)], writes=[('acc', tt_)])
                S.barrier()
                ar.release()
                g2b = ar.alloc([2, D], F32)
                hb3 = [ar.alloc([D], F32) for _ in range(2)]
                dma('sync', g2b[:, 0, :], GATE[l, 0:1, 1, :].partition_broadcast(128), writes=['g2b'])
                dma('sync', g2b[:, 1, :], GATE[l, 1:2, 1, :].partition_broadcast(128), writes=['g2b'])
                for tt_ in range(ntt):
                    t = b0 // 128 + tt_
                    r = 1 if t < 2 else 0
                    i = tt_ % 2
                    dma('sync', hb3[i], H[t * 128:(t + 1) * 128, :], reads=[('hb3', i), ('H', t)], writes=[('hb3', i)])
                    tt('dve', acc[:, tt_, :], acc[:, tt_, :], g2b[:, r, :], ALU.mult, reads=[('acc', tt_), 'g2b'], writes=[('acc', tt_)])
                    tt('dve', hb3[i], hb3[i], acc[:, tt_, :], ALU.add, reads=[('hb3', i), ('acc', tt_)], writes=[('hb3', i)])
                    if last:
                        dma('sync', OUT[(t - 2) * 128:(t - 1) * 128, :], hb3[i], reads=[('hb3', i)], writes=[U('OUT')])
                    else:
                        dma('sync', H[t * 128:(t + 1) * 128, :], hb3[i], reads=[('hb3', i)], writes=[('H', t)])
                S.barrier()
                ar.release()
        return

    with ExitStack() as st:
        try:
            body(st)
        except _Stop:
            pass
        S.emit(nc, st)
    return nc


def host_constants():
    ident = np.eye(128, dtype=np.float32)
    rotm = np.zeros((128, 128), np.float32)
    for j in range(128):
        blk = (j // 32) % 2
        if blk == 0:
            rotm[j + 32, j] = -1.0
        else:
            rotm[j - 32, j] = 1.0
    n_rows = TL // 64
    row = np.repeat(np.arange(n_rows), 64).astype(np.float32)
    col = np.tile(np.arange(64), n_rows).astype(np.float32)
    half = HD // 2
    inv = (1.0 / (np.float32(10000.0) ** (np.arange(0, half, 2, dtype=np.float32) / np.float32(half)))).astype(np.float32)
    ang_r = row[:, None] * inv
    ang_c = col[:, None] * inv
    ang = np.concatenate([ang_r, ang_r, ang_c, ang_c], axis=-1).astype(np.float32)
    cosT = np.ones((128, T), np.float32)
    sinT = np.zeros((128, T), np.float32)
    cosT[:, TC:] = np.cos(ang).T
    sinT[:, TC:] = np.sin(ang).T
    return {"ident": ident, "rotm": rotm, "cosT": np.ascontiguousarray(cosT), "sinT": np.ascontiguousarray(sinT)}


_NC_CACHE = {}


def kernel(**inputs):
    n = 8
    if 'nc' not in _NC_CACHE:
        _NC_CACHE['nc'] = build()
    nc = _NC_CACHE['nc']
    consts = host_constants()
    shared = {k: np.ascontiguousarray(np.asarray(inputs[k], dtype=np.float32)) for k in WEIGHT_NAMES}
    shared.update(consts)
    x = np.asarray(inputs["x"], dtype=np.float32)
    c = np.asarray(inputs["c"], dtype=np.float32)
    ctx = np.asarray(inputs["ctx"], dtype=np.float32)
    c_ctx = np.asarray(inputs["c_ctx"], dtype=np.float32)
    in_maps = []
    for b in range(n):
        m = dict(shared)
        m["x"] = np.ascontiguousarray(x[b])
        m["ctx"] = np.ascontiguousarray(ctx[b])
        m["c2"] = np.ascontiguousarray(np.stack([c[b], c_ctx], axis=0))
        in_maps.append(m)
    res = run_bass_kernel_spmd(nc, in_maps, core_ids=list(range(n)))
    return np.stack([r["out"] for r in res.results], axis=0).astype(np.float32)
```

```python
import math
import numpy as np
from contextlib import ExitStack
import concourse.bass as bass
import concourse.mybir as mybir
from concourse.bass_utils import run_bass_kernel_spmd

F32 = mybir.dt.float32
BF16 = mybir.dt.bfloat16
U8 = mybir.dt.uint8
AF = mybir.ActivationFunctionType
ALU = mybir.AluOpType
AX = mybir.AxisListType

ENGS = ['sync', 'act', 'dve', 'pool', 'pe']
NDMA = 8
EPOCH = 30000


class Op:
    __slots__ = ('eng', 'fn', 'deps', 'is_dma', 'sig', 'sigval', 'name')


class Sched:
    def __init__(self):
        self.ops = {e: [] for e in ENGS}
        self.tok_w = {}
        self.tok_r = {}
        self.last = {e: None for e in ENGS}
        self.dmas = {e: [] for e in ENGS}

    def add(self, eng, fn, reads=(), writes=(), dma=False, name=None):
        op = Op()
        op.eng = eng
        op.fn = fn
        op.is_dma = dma
        op.sig = dma
        op.sigval = None
        op.name = name
        deps = {}
        for t in reads:
            w = self.tok_w.get(t)
            if w is not None:
                deps[id(w)] = (w, 'raw')
        for t in writes:
            w = self.tok_w.get(t)
            if w is not None and id(w) not in deps:
                deps[id(w)] = (w, 'waw')
            for r in self.tok_r.get(t, ()):
                if id(r) not in deps:
                    deps[id(r)] = (r, 'war')
        dl = []
        for d, kind in deps.values():
            if (not d.is_dma) and (not dma) and d.eng == eng:
                if eng == 'pe':
                    continue
                if kind != 'raw':
                    continue
            dl.append(d)
        op.deps = dl
        for t in reads:
            self.tok_r.setdefault(t, []).append(op)
        for t in writes:
            self.tok_w[t] = op
            self.tok_r[t] = []
        self.ops[eng].append(op)
        if dma:
            self.dmas[eng].append(op)
        else:
            self.last[eng] = op
        return op

    def barrier(self):
        deps = []
        for e in ENGS:
            if self.last[e] is not None:
                deps.append(self.last[e])
            deps.extend(self.dmas[e][-NDMA:])
        for e in ENGS:
            op = Op()
            op.eng = e
            op.fn = None
            op.is_dma = False
            op.sig = False
            op.sigval = None
            op.name = 'barrier'
            op.deps = list(deps)
            self.ops[e].append(op)
        self.tok_w = {}
        self.tok_r = {}

    def emit(self, nc, stack):
        for e in ENGS:
            for op in self.ops[e]:
                for d in op.deps:
                    d.sig = True
        csem = {}
        for e in ENGS:
            n = sum(1 for op in self.ops[e] if op.sig and not op.is_dma)
            csem[e] = [stack.enter_context(nc.semaphore('c_%s_%d' % (e, i)))
                       for i in range(n // EPOCH + 1)]
        dsem = {}
        for e in ENGS:
            if self.dmas[e]:
                dsem[e] = [stack.enter_context(nc.semaphore('d_%s_%d' % (e, i)))
                           for i in range(NDMA)]
        for e in ENGS:
            k = 0
            j = 0
            for op in self.ops[e]:
                if op.is_dma:
                    op.sigval = (dsem[e][j % NDMA], 16 * (j // NDMA + 1), ('d', e, j % NDMA))
                    j += 1
                elif op.sig:
                    op.sigval = (csem[e][k // EPOCH], k % EPOCH + 1, ('c', e, k // EPOCH))
                    k += 1
        blk = stack.enter_context(nc.Block())
        hooks = {'sync': blk.sync, 'act': blk.scalar, 'dve': blk.vector,
                 'pool': blk.gpsimd, 'pe': blk.tensor}
        all_sigs = []
        for e in ENGS:
            for op in self.ops[e]:
                if op.sigval is not None:
                    all_sigs.append(op)

        def make(e):
            def body(eng):
                waited = {}

                def wait(sv):
                    sem, val, key = sv
                    if waited.get(key, 0) < val:
                        eng.wait_ge(sem, val)
                        waited[key] = val
                for op in self.ops[e]:
                    for d in op.deps:
                        wait(d.sigval)
                    if op.is_dma:
                        sem, val, key = op.sigval
                        if val > 16:
                            wait((sem, val - 16, key))
                    if op.fn is not None:
                        ins = op.fn(eng)
                        if op.sigval is not None:
                            sem, val, key = op.sigval
                            ins.then_inc(sem, 16 if op.is_dma else 1)
                if e == 'sync':
                    final = {}
                    for op in all_sigs:
                        sem, val, key = op.sigval
                        if key not in final or final[key][1] < val:
                            final[key] = (sem, val, key)
                    for sv in final.values():
                        wait(sv)
            return body
        for e in ENGS:
            hooks[e](make(e))


class Arena:
    def __init__(self, nc, stack, nbytes, name='arena'):
        self.t = stack.enter_context(nc.sbuf_tensor(name, [128, nbytes], U8))
        self.nbytes = nbytes
        self.off = 0
        self.marks = []
        self.peak = 0

    def alloc(self, shape, dtype, parts=128):
        esz = 2 if dtype == BF16 else 4
        n = int(np.prod(shape))
        nb = n * esz
        off = (self.off + 31) // 32 * 32
        assert off + nb <= self.nbytes, ('arena overflow', off + nb, self.nbytes)
        self.off = off + nb
        self.peak = max(self.peak, self.off)
        v = self.t[0:parts, off:off + nb].bitcast(dtype)
        if len(shape) > 1:
            names = ' '.join('d%d' % i for i in range(len(shape)))
            kw = {'d%d' % i: int(shape[i]) for i in range(len(shape))}
            v = v.rearrange('p (%s) -> p %s' % (names, names), **kw)
        return v

    def mark(self):
        self.marks.append(self.off)

    def release(self):
        self.off = self.marks.pop()


D = 2048
KD = 16
TC = 256
TL = 2048
T = TC + TL
NT = T // 128
DEPTH = 2
HD = 128
EPS = 1e-6
NEXP = 32
DEXP = 512
IN_COLS = 12800
BLKS = [(0, 256), (256, 512), (768, 512), (1280, 512), (1792, 512)]

F_N1G, F_N2G, F_BG, F_CW, F_CB = 0, 16, 32, 80, 112
F_BA, F_BX, F_LAM, F_QNA, F_KNA, F_QNC, F_KNC, F_SUB = 128, 144, 160, 176, 177, 178, 179, 180

WEIGHT_NAMES = ["w_ada", "b_ada", "norm1_g", "norm2_g", "w_in", "b_gate", "q_norm_a", "k_norm_a",
                "diff_lambda", "sub_norm_a", "q_norm_c", "k_norm_c", "conv_w", "conv_b", "lru_w_a",
                "lru_b_a", "lru_w_x", "lru_b_x", "lru_lambda", "w_branch_a", "w_branch_b",
                "w_branch_c", "w_out", "w_group", "b_group", "w_route", "b_route", "w1", "w3", "w2"]
WEIGHT_SHAPES = {
    "w_ada": [2, 2048, 12288], "b_ada": [2, 12288], "norm1_g": [2, 2048], "norm2_g": [2, 2048],
    "w_in": [2, 2048, 12800], "b_gate": [2, 3, 2048], "q_norm_a": [2, 128], "k_norm_a": [2, 128],
    "diff_lambda": [2, 4, 128], "sub_norm_a": [2, 256], "q_norm_c": [2, 128], "k_norm_c": [2, 128],
    "conv_w": [2, 4, 1024], "conv_b": [2, 1024], "lru_w_a": [2, 2, 8, 128, 128], "lru_b_a": [2, 2, 1024],
    "lru_w_x": [2, 2, 8, 128, 128], "lru_b_x": [2, 2, 1024], "lru_lambda": [2, 2, 1024],
    "w_branch_a": [2, 1024, 2048], "w_branch_b": [2, 1024, 2048], "w_branch_c": [2, 1024, 2048],
    "w_out": [2, 2048, 2048], "w_group": [2, 2048, 4], "b_group": [2, 4], "w_route": [2, 2048, 32],
    "b_route": [2, 32], "w1": [2, 32, 2048, 512], "w3": [2, 32, 2048, 512], "w2": [2, 32, 512, 2048],
}


class _Stop(Exception):
    pass


def build(dbg=False, phases=None, nlayers=DEPTH, stop=None):
    nc = bass.Bass("TRN2", target_bir_lowering=False)
    IN = {}
    IN["x"] = nc.dram_tensor("x", [TL, D], F32, kind="ExternalInput").ap()
    IN["ctx"] = nc.dram_tensor("ctx", [TC, D], F32, kind="ExternalInput").ap()
    IN["c2"] = nc.dram_tensor("c2", [2, D], F32, kind="ExternalInput").ap()
    for n in WEIGHT_NAMES:
        IN[n] = nc.dram_tensor(n, WEIGHT_SHAPES[n], F32, kind="ExternalInput").ap()
    IN["ident"] = nc.dram_tensor("ident", [128, 128], F32, kind="ExternalInput").ap()
    IN["rotm"] = nc.dram_tensor("rotm", [128, 128], F32, kind="ExternalInput").ap()
    IN["cosT"] = nc.dram_tensor("cosT", [128, T], F32, kind="ExternalInput").ap()
    IN["sinT"] = nc.dram_tensor("sinT", [128, T], F32, kind="ExternalInput").ap()
    OUT = nc.dram_tensor("out", [TL, D], F32, kind="ExternalOutput").ap()
    sk = "ExternalOutput" if dbg else "Internal"

    def scratch(name, shape, dt):
        return nc.dram_tensor(name, shape, dt, kind=sk).ap()
    H = scratch("H", [T, D], F32)
    GATE = scratch("GATE", [DEPTH, 2, 2, D], F32)
    QA = scratch("QA", [8, 128, T], BF16)
    KA = scratch("KA", [8, 128, T], BF16)
    QC = scratch("QC", [8, 128, T], BF16)
    KC = scratch("KC", [2, 128, T], BF16)
    VA = scratch("VA", [T, 1024], BF16)
    VC = scratch("VC", [T, 256], BF16)
    BX = scratch("BX", [8, 128, T], F32)
    BYG = scratch("BYG", [8, 128, T], BF16)
    G = scratch("G", [48, 128, T], BF16)
    YA = scratch("YA", [8, 128, T], BF16)
    YB = scratch("YB", [8, 128, T], BF16)
    YC = scratch("YC", [8, 128, T], BF16)
    MT = scratch("MT", [16, 128, T], BF16)
    VT = scratch("VT", [16, 128, T], BF16)
    CMB = scratch("CMB", [NEXP, T], F32)

    S = Sched()
    uid = [0]

    def U(prefix):
        uid[0] += 1
        return (prefix, uid[0])

    def chk(name):
        if stop == name:
            raise _Stop()

    def body(st):
        ar = Arena(nc, st, 200 * 1024)
        psum = st.enter_context(nc.psum_tensor("ps", [128, 4096], F32))

        def bank(b, n=512, parts=128):
            return psum[0:parts, b * 512:b * 512 + n]

        def PS(b):
            return ('ps', b)

        def dma(q, out, in_, reads=(), writes=()):
            return S.add(q, lambda e: e.dma_start(out=out, in_=in_), reads, writes, dma=True)

        def act(out, in_, func, reads, writes, bias=None, scale=None, accum=None):
            kw = {}
            if bias is not None:
                kw['bias'] = bias
            if scale is not None:
                kw['scale'] = scale
            if accum is not None:
                kw['accum_out'] = accum
            return S.add('act', lambda e: e.activation(out=out, in_=in_, func=func, **kw), reads, writes)

        def tt(eng, out, a, b, op, reads, writes):
            return S.add(eng, lambda e: e.tensor_tensor(out=out, in0=a, in1=b, op=op), reads, writes)

        def ts(eng, out, a, s1, s2, op0, op1, reads, writes):
            if s2 is None:
                return S.add(eng, lambda e: e.tensor_scalar(out=out, in0=a, scalar1=s1, scalar2=None, op0=op0), reads, writes)
            return S.add(eng, lambda e: e.tensor_scalar(out=out, in0=a, scalar1=s1, scalar2=s2, op0=op0, op1=op1), reads, writes)

        def stt(out, a, s, b, op0, op1, reads, writes):
            return S.add('dve', lambda e: e.scalar_tensor_tensor(out=out, in0=a, scalar=s, in1=b, op0=op0, op1=op1), reads, writes)

        def cp(eng, out, in_, reads, writes):
            return S.add(eng, lambda e: e.tensor_copy(out=out, in_=in_), reads, writes)

        def recip(out, in_, reads, writes):
            return S.add('dve', lambda e: e.reciprocal(out=out, in_=in_), reads, writes)

        def mms(out, pairs, reads, writes):
            def f(e):
                n = len(pairs)
                ins = None
                for i, (l, r) in enumerate(pairs):
                    ins = e.matmul(out, l, r, start=(i == 0), stop=(i == n - 1))
                return ins
            return S.add('pe', f, reads, writes)

        def transposes(items, ident, reads, writes):
            def f(e):
                ins = None
                for (o, i_) in items:
                    ins = e.transpose(out=o, in_=i_, identity=ident)
                return ins
            return S.add('pe', f, reads, writes)

        ident_f = ar.alloc([128], F32)
        ident_b = ar.alloc([128], BF16)
        ones_b = ar.alloc([128], BF16)
        rotm = ar.alloc([128], BF16)
        featT = ar.alloc([DEPTH, 2, 128], F32)
        modT = ar.alloc([DEPTH, 96, 2], F32)
        AB = ar.alloc([DEPTH, 2, 4, 16], F32)
        lamv = ar.alloc([DEPTH, 4], F32)
        lruS = ar.alloc([DEPTH, 2, 16], F32)
        epsc = ar.alloc([4], F32)
        dma('sync', ident_f, IN["ident"], writes=['ident_f'])
        dma('pool', ident_b, IN["ident"], writes=['ident_b'])
        dma('pool', rotm, IN["rotm"], writes=['rotm'])
        S.add('pool', lambda e: e.memset(ones_b, 1.0), writes=['ones_b'])
        S.add('pool', lambda e: e.memset(epsc, EPS), writes=['epsc'])
        eps_ap = epsc[:, 0:1]

        ar.mark()
        stg = ar.alloc([2, 128], F32)
        for l in range(DEPTH):
            S.add('pool', lambda e: e.memset(stg, 0.0), writes=['stg'])
            def ld(off, n, src):
                tl, o = divmod(off, 128)
                dma('sync', stg[o:o + n, tl, :], src, reads=['stg'], writes=[U('stgp')])
            ld(F_N1G, 16, IN["norm1_g"][l].rearrange("(n p) -> n p", p=128))
            ld(F_N2G, 16, IN["norm2_g"][l].rearrange("(n p) -> n p", p=128))
            ld(F_BG, 48, IN["b_gate"][l].rearrange("a (n p) -> (a n) p", p=128))
            ld(F_CW, 32, IN["conv_w"][l].rearrange("a (n p) -> (a n) p", p=128))
            ld(F_CB, 8, IN["conv_b"][l].rearrange("(n p) -> n p", p=128))
            ld(F_BA, 16, IN["lru_b_a"][l].rearrange("a (n p) -> (a n) p", p=128))
            ld(F_BX, 16, IN["lru_b_x"][l].rearrange("a (n p) -> (a n) p", p=128))
            ld(F_LAM, 16, IN["lru_lambda"][l].rearrange("a (n p) -> (a n) p", p=128))
            ld(F_QNA, 1, IN["q_norm_a"][l:l + 1, :])
            ld(F_KNA, 1, IN["k_norm_a"][l:l + 1, :])
            ld(F_QNC, 1, IN["q_norm_c"][l:l + 1, :])
            ld(F_KNC, 1, IN["k_norm_c"][l:l + 1, :])
            ld(F_SUB, 2, IN["sub_norm_a"][l].rearrange("(n p) -> n p", p=128))
            S.barrier()
            transposes([(bank(0, 128), stg[:, 0, :]), (bank(1, 128), stg[:, 1, :])], ident_f,
                       reads=['stg', 'ident_f'], writes=[PS(0), PS(1)])
            cp('dve', featT[:, l, 0, :], bank(0, 128), reads=[PS(0)], writes=['featT'])
            cp('dve', featT[:, l, 1, :], bank(1, 128), reads=[PS(1)], writes=['featT'])
            S.barrier()
        ar.release()

        if dbg:
            FEAT = nc.dram_tensor('FEAT', [128, DEPTH * 2 * 128], F32, kind='ExternalOutput').ap()
            dma('sync', FEAT, featT.rearrange('p a b c -> p (a b c)'), writes=[U('FEAT')])
        chk('F')

        def FT(l, ch):
            tl, o = divmod(ch, 128)
            return featT[:, l, tl, o:o + 1]

        ar.mark()
        c2t = ar.alloc([D], F32, parts=2)
        sct = ar.alloc([D], F32, parts=2)
        sc2 = ar.alloc([KD, 2], BF16)
        brow = ar.alloc([6 * D], F32, parts=2)
        modrow = ar.alloc([6 * D], F32, parts=2)
        wa = [ar.alloc([KD, 512], BF16) for _ in range(3)]
        dl = ar.alloc([4, 128], F32)
        pr = ar.alloc([2, 128], F32)
        sm = ar.alloc([2], F32)
        dma('sync', c2t, IN["c2"], writes=['c2t'])
        act(sct, c2t, AF.Silu, reads=['c2t'], writes=['sct'])
        pv = bank(0, 32).rearrange("p (k r) -> p k r", r=2)
        transposes([(pv[:, k, :], sct[0:2, k * 128:(k + 1) * 128]) for k in range(KD)], ident_f[0:2, 0:2],
                   reads=['sct', 'ident_f'], writes=[PS(0)])
        cp('dve', sc2, pv, reads=[PS(0)], writes=['sc2'])
        for l in range(nlayers):
            dma('sync', brow, IN["b_ada"][l:l + 1, :].partition_broadcast(2), reads=['brow'], writes=['brow'])
            for g in range(24):
                wt = wa[g % 3]
                dma('pool', wt, IN["w_ada"][l][:, g * 512:(g + 1) * 512].rearrange("(k p) c -> p k c", p=128),
                    reads=[('wa', g % 3)], writes=[('wa', g % 3)])
                pb = 1 + g % 2
                mms(bank(pb, 512, 2), [(sc2[:, k, :], wt[:, k, :]) for k in range(KD)],
                    reads=['sc2', ('wa', g % 3)], writes=[PS(pb)])
                tt('dve', modrow[:, g * 512:(g + 1) * 512], bank(pb, 512, 2), brow[:, g * 512:(g + 1) * 512], ALU.add,
                   reads=[PS(pb), 'brow'], writes=['modrow'])
            dma('sync', GATE[l, :, 0, :], modrow[:, 2 * D:3 * D], reads=['modrow'], writes=[('GATE', l)])
            dma('sync', GATE[l, :, 1, :], modrow[:, 5 * D:6 * D], reads=['modrow'], writes=[('GATE', l)])
            pm = bank(3, 192).rearrange("p (j r) -> p j r", r=2)
            transposes([(pm[:, j, :], modrow[0:2, j * 128:(j + 1) * 128]) for j in range(96)], ident_f[0:2, 0:2],
                       reads=['modrow', 'ident_f'], writes=[PS(3)])
            cp('dve', modT[:, l, :, :], pm, reads=[PS(3)], writes=['modT'])
            for r in range(2):
                ts('dve', AB[:, l, r, 0, :], modT[:, l, 16:32, r], 1.0, None, ALU.add, None, reads=['modT'], writes=['AB'])
                tt('dve', AB[:, l, r, 0, :], AB[:, l, r, 0, :], featT[:, l, 0, F_N1G:F_N1G + 16], ALU.mult, reads=['AB', 'featT'], writes=['AB'])
                cp('dve', AB[:, l, r, 1, :], modT[:, l, 0:16, r], reads=['modT'], writes=['AB'])
                ts('dve', AB[:, l, r, 2, :], modT[:, l, 64:80, r], 1.0, None, ALU.add, None, reads=['modT'], writes=['AB'])
                tt('dve', AB[:, l, r, 2, :], AB[:, l, r, 2, :], featT[:, l, 0, F_N2G:F_N2G + 16], ALU.mult, reads=['AB', 'featT'], writes=['AB'])
                cp('dve', AB[:, l, r, 3, :], modT[:, l, 48:64, r], reads=['modT'], writes=['AB'])
            dma('sync', dl, IN["diff_lambda"][l:l + 1].partition_broadcast(128), reads=['dl'], writes=['dl'])
            tt('dve', pr, dl[:, 0:4:2, :], dl[:, 1:4:2, :], ALU.mult, reads=['dl'], writes=['pr'])
            S.add('dve', lambda e, sm=sm, pr=pr: e.tensor_reduce(out=sm, in_=pr, axis=AX.X, op=ALU.add), reads=['pr'], writes=['sm'])
            act(sm, sm, AF.Exp, reads=['sm'], writes=['sm'])
            lam_init = 0.8 - 0.6 * math.exp(-0.3 * l)
            tt('dve', lamv[:, l, 0:1], sm[:, 1:2], sm[:, 0:1], ALU.subtract, reads=['sm'], writes=['lamv'])
            ts('dve', lamv[:, l, 0:1], lamv[:, l, 0:1], -lam_init, None, ALU.add, None, reads=['lamv'], writes=['lamv'])
            act(lruS[:, l, 0, :], featT[:, l, 1, F_LAM - 128:F_LAM - 128 + 16], AF.Exp, reads=['featT'], writes=['lruS'], scale=-1.0)
            ts('dve', lruS[:, l, 0, :], lruS[:, l, 0, :], 1.0, None, ALU.add, None, reads=['lruS'], writes=['lruS'])
            act(lruS[:, l, 0, :], lruS[:, l, 0, :], AF.Ln, reads=['lruS'], writes=['lruS'])
            ts('dve', lruS[:, l, 1, :], lruS[:, l, 0, :], -16.0, None, ALU.mult, None, reads=['lruS'], writes=['lruS'])
            ts('dve', lruS[:, l, 0, :], lruS[:, l, 0, :], -8.0, None, ALU.mult, None, reads=['lruS'], writes=['lruS'])
            S.barrier()
        ar.release()
        S.barrier()

        chk('0')

        def hsrc(l, t):
            if l == 0:
                return IN["ctx"][t * 128:(t + 1) * 128, :] if t < 2 else IN["x"][(t - 2) * 128:(t - 1) * 128, :]
            return H[t * 128:(t + 1) * 128, :]

        def nm_A(hbuf, hb_tok, junk, ss, xn, i):
            act(junk, hbuf, AF.Square, reads=[hb_tok], writes=[('junk', i), ('ss', i)], accum=ss[:, 0:1])
            ts('dve', ss[:, 1:2], ss[:, 0:1], 1.0 / D, EPS, ALU.mult, ALU.add, reads=[('ss', i)], writes=[('ss', i)])
            act(ss[:, 2:3], ss[:, 1:2], AF.Ln, reads=[('ss', i)], writes=[('ss', i)])
            act(ss[:, 3:4], ss[:, 2:3], AF.Exp, reads=[('ss', i)], writes=[('ss', i)], scale=-0.5)
            act(xn, hbuf, AF.Identity, reads=[hb_tok, ('ss', i)], writes=[('xn', i)], scale=ss[:, 3:4])

        def nm_B(l, t, which, dst_fn, xn, pst, pst_tok, i):
            r = 1 if t < 2 else 0
            transposes([(pst[:, c, :], xn[:, c * 128:(c + 1) * 128]) for c in range(KD)], ident_b,
                       reads=[('xn', i), 'ident_b'], writes=pst_tok)
            for c in range(KD):
                d_ap, d_tok = dst_fn(c)
                ts('dve', d_ap, pst[:, c, :], AB[:, l, r, which, c:c + 1], AB[:, l, r, which + 1, c:c + 1], ALU.mult, ALU.add,
                   reads=pst_tok + ['AB'], writes=[d_tok])

        def pst_view(b0):
            return psum[:, b0 * 512:(b0 + 2) * 512].bitcast(BF16).rearrange("p (c t) -> p c t", t=128)

        for l in range(nlayers):
            last = (l == DEPTH - 1)
            lam_init = 0.8 - 0.6 * math.exp(-0.3 * l)
            blocks = BLKS
            ar.mark()
            uT = ar.alloc([KD, T], BF16)
            ar.mark()
            hb = [ar.alloc([D], F32) for _ in range(2)]
            junk = ar.alloc([D], BF16)
            xn = [ar.alloc([D], BF16) for _ in range(2)]
            ssb = [ar.alloc([4], F32) for _ in range(2)]
            def A1(t):
                i = t % 2
                dma('sync', hb[i], hsrc(l, t), reads=[('hb', i)], writes=[('hb', i)])
                nm_A(hb[i], ('hb', i), junk, ssb[i], xn[i], i)

            def B1(t):
                i = t % 2
                nm_B(l, t, 0, lambda c, t=t: (uT[:, c, t * 128:(t + 1) * 128], ('uT', t)),
                     xn[i], pst_view(2 * i), [PS(2 * i), PS(2 * i + 1)], i)
            A1(0)
            for t in range(NT):
                if t + 1 < NT:
                    A1(t + 1)
                B1(t)
            S.barrier()
            ar.release()
            chk('1a')
            wg = [ar.alloc([KD, 512], BF16) for _ in range(3)]
            cosT = ar.alloc([T], F32)
            sinT = ar.alloc([T], F32)
            stage_b = [ar.alloc([4, 512], BF16) for _ in range(2)]
            stage_f = [ar.alloc([4, 512], F32) for _ in range(2)]
            stage_v = [ar.alloc([512], BF16) for _ in range(2)]
            sqb = [ar.alloc([512], BF16) for _ in range(3)]
            xgb = [ar.alloc([512], BF16) for _ in range(3)]
            sdt = [ar.alloc([512], F32) for _ in range(3)]
            t1t = [ar.alloc([512], F32) for _ in range(3)]
            t2t = [ar.alloc([512], F32) for _ in range(3)]
            pend = []
            dma('sync', cosT, IN["cosT"], writes=['cosT'])
            dma('sync', sinT, IN["sinT"], writes=['sinT'])
            cnt = {'pb': 0, 'q': 0, 'st': 0, 'sv': 0}
            ut_all = [('uT', t) for t in range(NT)]

            import os as _os
            def qpath(P0, pb, n, t0, gq, dst, dtok):
                qi = cnt['q'] % 3
                pi_ = cnt['q'] % 2
                cnt['q'] += 1
                act(t2t[qi][:, 0:n], P0, AF.Copy, reads=[PS(pb)], writes=[('t2t', qi)])
                act(sqb[qi][:, 0:n], t2t[qi][:, 0:n], AF.Square, reads=[('t2t', qi)], writes=[('sqb', qi)])
                ts('dve', xgb[qi][:, 0:n], t2t[qi][:, 0:n], gq, None, ALU.mult, None, reads=[('t2t', qi), 'featT'], writes=[('xgb', qi)])

                def post():
                    p1, p2 = 3 + pi_, 5 + pi_
                    mms(bank(p1, n), [(ones_b, sqb[qi][:, 0:n])], reads=['ones_b', ('sqb', qi)], writes=[PS(p1)])
                    mms(bank(p2, n), [(rotm, xgb[qi][:, 0:n])], reads=['rotm', ('xgb', qi)], writes=[PS(p2)])
                    act(sdt[qi][:, 0:n], bank(p1, n), AF.Ln, reads=[PS(p1), 'epsc'], writes=[('sdt', qi)], bias=eps_ap, scale=1.0 / HD)
                    act(sdt[qi][:, 0:n], sdt[qi][:, 0:n], AF.Exp, reads=[('sdt', qi)], writes=[('sdt', qi)], scale=-0.5)
                    tt('pool', t1t[qi][:, 0:n], xgb[qi][:, 0:n], cosT[:, t0:t0 + n], ALU.mult, reads=[('xgb', qi), 'cosT'], writes=[('t1t', qi)])
                    tt('dve', t2t[qi][:, 0:n], bank(p2, n), sinT[:, t0:t0 + n], ALU.mult, reads=[PS(p2), 'sinT'], writes=[('t2t', qi)])
                    tt('pool', t1t[qi][:, 0:n], t1t[qi][:, 0:n], t2t[qi][:, 0:n], ALU.add, reads=[('t1t', qi), ('t2t', qi)], writes=[('t1t', qi)])
                    tt('dve', dst, t1t[qi][:, 0:n], sdt[qi][:, 0:n], ALU.mult, reads=[('t1t', qi), ('sdt', qi)], writes=[dtok])
                return post

            def flush():
                while pend:
                    pend.pop(0)()

            _gl = [int(a) for a in _os.environ['DBG_GROUPS'].split(',')] if 'DBG_GROUPS' in _os.environ else list(range(25))
            for g in _gl:
                if g < 2:
                    kind, dstT, base, gq = 'q', QA, g * 4, FT(l, F_QNA)
                elif g < 4:
                    kind, dstT, base, gq = 'q', KA, (g - 2) * 4, FT(l, F_KNA)
                elif g < 6:
                    kind = 'v'
                elif g < 8:
                    kind, dstT, base = 'bx', BX, (g - 6) * 4
                elif g < 10:
                    kind, dstT, base = 'by', BYG, (g - 8) * 4
                elif g < 12:
                    kind, dstT, base, gq = 'q', QC, (g - 10) * 4, FT(l, F_QNC)
                elif g == 12:
                    kind = 'ckv'
                else:
                    kind, dstT, base = 'gate', G, (g - 13) * 4
                need_ctx = (not last) or kind in ('v', 'bx', 'ckv') or (kind == 'q' and dstT is KA)
                wi = g % 3
                dma('pool', wg[wi], IN["w_in"][l][:, g * 512:(g + 1) * 512].rearrange("(k p) c -> p k c", p=128),
                    reads=[('wg', wi)], writes=[('wg', wi)])
                for (t0, n) in blocks:
                    if t0 == 0 and not need_ctx:
                        continue
                    if kind in ('v', 'ckv'):
                        c0, nc_ = (0, 512) if kind == 'v' else (256, 256)
                        for tt_ in range(t0 // 128, (t0 + n) // 128):
                            pb = cnt['pb'] % 3
                            cnt['pb'] += 1
                            mms(bank(pb, nc_), [(uT[:, k, tt_ * 128:(tt_ + 1) * 128], wg[wi][:, k, c0:c0 + nc_]) for k in range(KD)],
                                reads=[('uT', tt_), ('wg', wi)], writes=[PS(pb)])
                            si = cnt['sv'] % 2
                            cnt['sv'] += 1
                            act(stage_v[si][:, 0:nc_], bank(pb, nc_), AF.Copy, reads=[PS(pb)], writes=[('sv', si)])
                            if kind == 'v':
                                dma('sync', VA[tt_ * 128:(tt_ + 1) * 128, (g - 4) * 512:(g - 3) * 512], stage_v[si], reads=[('sv', si)], writes=[U('VA')])
                            else:
                                dma('sync', VC[tt_ * 128:(tt_ + 1) * 128, :], stage_v[si][:, 0:256], reads=[('sv', si)], writes=[U('VC')])
                    ncc = 2 if kind == 'ckv' else (0 if kind == 'v' else 4)
                    if ncc == 0:
                        continue
                    si = cnt['st'] % 2
                    cnt['st'] += 1
                    utk = [('uT', t_) for t_ in range(t0 // 128, (t0 + n) // 128)]
                    for cc in range(ncc):
                        pb = cnt['pb'] % 3
                        cnt['pb'] += 1
                        P0 = bank(pb, n)
                        mms(P0, [(wg[wi][:, k, cc * 128:(cc + 1) * 128], uT[:, k, t0:t0 + n]) for k in range(KD)],
                            reads=utk + [('wg', wi)], writes=[PS(pb)])
                        if kind == 'bx':
                            act(stage_f[si][:, cc, 0:n], P0, AF.Copy, reads=[PS(pb)], writes=[('stf', si)])
                        elif kind == 'by':
                            act(stage_b[si][:, cc, 0:n], P0, AF.Gelu_apprx_tanh, reads=[PS(pb)], writes=[('stb', si)])
                        elif kind == 'gate':
                            act(stage_b[si][:, cc, 0:n], P0, AF.Sigmoid, reads=[PS(pb), 'featT'], writes=[('stb', si)],
                                bias=FT(l, F_BG + base + cc))
                        elif kind == 'q':
                            post = qpath(P0, pb, n, t0, gq, stage_b[si][:, cc, 0:n], ('stb', si))
                            flush()
                            pend.append(post)
                        elif kind == 'ckv':
                            post = qpath(P0, pb, n, t0, FT(l, F_KNC), stage_b[si][:, cc, 0:n], ('stb', si))
                            flush()
                            pend.append(post)
                        if kind not in ('q', 'ckv'):
                            flush()
                    if kind == 'bx':
                        dma('sync', BX[base:base + 4, :, t0:t0 + n].rearrange("c p t -> p c t"), stage_f[si][:, :, 0:n],
                            reads=[('stf', si)], writes=[U('BX')])
                    elif kind == 'ckv':
                        pend.append(lambda si=si, t0=t0, n=n: dma('sync', KC[:, :, t0:t0 + n].rearrange("c p t -> p c t"), stage_b[si][:, 0:2, 0:n],
                                                                 reads=[('stb', si)], writes=[U('KC')]))
                    elif kind == 'q':
                        pend.append(lambda si=si, t0=t0, n=n, dstT=dstT, base=base: dma('sync', dstT[base:base + 4, :, t0:t0 + n].rearrange("c p t -> p c t"),
                                                                                      stage_b[si][:, :, 0:n], reads=[('stb', si)], writes=[U('dst')]))
                    else:
                        dma('sync', dstT[base:base + 4, :, t0:t0 + n].rearrange("c p t -> p c t"), stage_b[si][:, :, 0:n],
                            reads=[('stb', si)], writes=[U('dst')])
            flush()
            S.barrier()
            ar.release()
            if phases is not None and 2 not in phases:
                continue

            ar.mark()
            KAs = ar.alloc([8, T], BF16)
            VAs = ar.alloc([NT, 1024], BF16)
            KCs = ar.alloc([2, T], BF16)
            VCs = ar.alloc([NT, 256], BF16)
            qb_ = [ar.alloc([512], BF16) for _ in range(2)]
            ptile = [ar.alloc([2, 512], BF16) for _ in range(3)]
            rden = ar.alloc([512], F32)
            lnd = ar.alloc([512], F32)
            oraw = [ar.alloc([3, 512], F32) for _ in range(2)]
            tailq = []
            tailq2 = []
            on_ = ar.alloc([2, 2, 512], F32)
            ot = ar.alloc([2, 512], F32)
            sq2 = ar.alloc([2, 512], BF16)
            rs_ = ar.alloc([512], F32)
            sga = ar.alloc([2], F32)
            yst = [ar.alloc([2, 512], BF16) for _ in range(2)]
            dma('sync', KAs, KA.rearrange("h p t -> p h t"), writes=['KAs'])
            dma('sync', VAs, VA.rearrange("(k p) c -> p k c", p=128), writes=['VAs'])
            dma('sync', KCs, KC.rearrange("h p t -> p h t"), writes=['KCs'])
            dma('sync', VCs, VC.rearrange("(k p) c -> p k c", p=128), writes=['VCs'])
            ts('dve', sga, featT[:, l, 1, F_SUB - 128:F_SUB - 128 + 2], 1.0 - lam_init, None, ALU.mult, None, reads=['featT'], writes=['sga'])
            sc_ = float(HD) ** -0.5
            cnt2 = {'q': 0, 'p': 0, 'y': 0, 'o': 0}
            qblocks = [(256, 512, 0, NT), (768, 512, 0, NT), (1280, 512, 0, NT), (1792, 512, 0, NT)]
            if not last:
                qblocks = [(0, 256, 0, 2)] + qblocks

            def attn_map(Qsrc, Ks, Vlist, t0, n, k0, k1):
                qi = cnt2['q'] % 2
                cnt2['q'] += 1
                dma('sync', qb_[qi][:, 0:n], Qsrc[:, t0:t0 + n], reads=[('qb', qi)], writes=[('qb', qi)])
                kts = list(range(k0, k1))

                assert len(kts) % 2 == 0
                nst = len(kts) // 2
                PAIRS = [(0, 1), (5, 6)]

                def pair_view(p0):
                    return psum[:, p0 * 512:(p0 + 2) * 512].rearrange("p (b c) -> p b c", c=512)[:, :, 0:n]

                def score(s_):
                    p0, p1 = PAIRS[s_ % 2]
                    ka, kb = kts[2 * s_], kts[2 * s_ + 1]

                    def f(e):
                        e.matmul(bank(p0, n), Ks[:, ka * 128:(ka + 1) * 128], qb_[qi][:, 0:n], start=True, stop=True)
                        return e.matmul(bank(p1, n), Ks[:, kb * 128:(kb + 1) * 128], qb_[qi][:, 0:n], start=True, stop=True)
                    S.add('pe', f, reads=['K', ('qb', qi)], writes=[PS(p0), PS(p1)])
                score(0)
                if nst > 1:
                    score(1)
                for s_ in range(nst):
                    p0, p1 = PAIRS[s_ % 2]
                    pi = cnt2['p'] % 3
                    cnt2['p'] += 1
                    act(ptile[pi][:, :, 0:n], pair_view(p0), AF.Exp, reads=[PS(p0), PS(p1)], writes=[('pt', pi)], scale=sc_)
                    if s_ == 0:
                        while tailq:
                            tailq.pop(0)()
                    if s_ == min(3, nst - 1):
                        while tailq2:
                            tailq2.pop(0)()

                    def pvf(e, s_=s_, pi=pi):
                        ins = None
                        for h_ in range(2):
                            j = 2 * s_ + h_
                            kt = kts[j]
                            for (vf, b_) in Vlist:
                                ins = e.matmul(bank(b_, n), vf(kt), ptile[pi][:, h_, 0:n], start=(j == 0), stop=(j == len(kts) - 1))
                            ins = e.matmul(bank(4, n), ones_b, ptile[pi][:, h_, 0:n], start=(j == 0), stop=(j == len(kts) - 1))
                        return ins
                    S.add('pe', pvf, reads=[('pt', pi), 'V', 'ones_b'], writes=[PS(b_) for (_, b_) in Vlist] + [PS(4)])
                    if s_ + 2 < nst:
                        score(s_ + 2)
                oi = cnt2['o'] % 2
                cnt2['o'] += 1
                for ci, (_, b_) in enumerate(Vlist):
                    cp('dve', oraw[oi][:, ci, 0:n], bank(b_, n), reads=[PS(b_)], writes=[('oraw', oi)])
                cp('dve', oraw[oi][:, 2, 0:n], bank(4, n), reads=[PS(4)], writes=[('oraw', oi)])

                def norm():
                    act(lnd[:, 0:n], oraw[oi][:, 2, 0:n], AF.Ln, reads=[('oraw', oi)], writes=['lnd'])
                    act(rden[:, 0:n], lnd[:, 0:n], AF.Exp, reads=['lnd'], writes=['rden'], scale=-1.0)
                tailq.append(norm)
                return oi

            for (t0, n, k0, k1) in qblocks:
                for h in range(4):
                    for j in range(2):
                        i = 2 * h + j
                        oi = attn_map(QA[i], KAs[:, i, :], [(lambda kt, h=h: VAs[:, kt, h * 256:h * 256 + 128], 2),
                                                             (lambda kt, h=h: VAs[:, kt, h * 256 + 128:h * 256 + 256], 3)], t0, n, k0, k1)
                        def normA(j=j, oi=oi, n=n):
                            for c in range(2):
                                tt('dve', on_[:, j, c, 0:n], oraw[oi][:, c, 0:n], rden[:, 0:n], ALU.mult, reads=[('oraw', oi), 'rden'], writes=[('on', j)])
                        tailq.append(normA)

                    def headA(h=h, n=n, t0=t0):
                        for c in range(2):
                            stt(ot[:, c, 0:n], on_[:, 1, c, 0:n], lamv[:, l, 0:1], on_[:, 0, c, 0:n], ALU.mult, ALU.add,
                                reads=[('on', 0), ('on', 1), 'lamv'], writes=['ot'])
                        tt('pool', sq2[:, :, 0:n], ot[:, :, 0:n], ot[:, :, 0:n], ALU.mult, reads=['ot'], writes=['sq2'])
                        tailq2.append(lambda: headA2(h, n, t0))

                    def headA2(h, n, t0):
                        mms(bank(7, n), [(ones_b, sq2[:, 0, 0:n]), (ones_b, sq2[:, 1, 0:n])], reads=['sq2', 'ones_b'], writes=[PS(7)])
                        act(rs_[:, 0:n], bank(7, n), AF.Ln, reads=[PS(7), 'epsc'], writes=['rs'], bias=eps_ap, scale=1.0 / 256)
                        act(rs_[:, 0:n], rs_[:, 0:n], AF.Exp, reads=['rs'], writes=['rs'], scale=-0.5)
                        yi = cnt2['y'] % 2
                        cnt2['y'] += 1
                        for c in range(2):
                            stt(yst[yi][:, c, 0:n], ot[:, c, 0:n], sga[:, c:c + 1], rs_[:, 0:n], ALU.mult, ALU.mult,
                                reads=['ot', 'sga', 'rs'], writes=[('yst', yi)])
                        dma('sync', YA[2 * h:2 * h + 2, :, t0:t0 + n].rearrange("c p t -> p c t"), yst[yi][:, :, 0:n],
                            reads=[('yst', yi)], writes=[U('YA')])
                    tailq.append(headA)
                for hq in range(8):
                    kv = hq // 4
                    oi = attn_map(QC[hq], KCs[:, kv, :], [(lambda kt, kv=kv: VCs[:, kt, kv * 128:(kv + 1) * 128], 2)], t0, n, k0, k1)
                    def normC(hq=hq, oi=oi, n=n, t0=t0):
                        yi = cnt2['y'] % 2
                        cnt2['y'] += 1
                        tt('dve', yst[yi][:, 0, 0:n], oraw[oi][:, 0, 0:n], rden[:, 0:n], ALU.mult, reads=[('oraw', oi), 'rden'], writes=[('yst', yi)])
                        dma('sync', YC[hq, :, t0:t0 + n], yst[yi][:, 0, 0:n], reads=[('yst', yi)], writes=[U('YC')])
                    tailq.append(normC)
            while tailq:
                tailq.pop(0)()
            while tailq2:
                tailq2.pop(0)()
            S.barrier()
            ar.release()
            if phases is not None and 3 not in phases:
                continue

            ar.mark()
            lw = ar.alloc([2, 2, 8, 128], BF16)
            bxt = ar.alloc([T], F32)
            xc = ar.alloc([T], F32)
            xcb = ar.alloc([T], BF16)
            rr = ar.alloc([T], F32)
            ii = ar.alloc([T], F32)
            aa = ar.alloc([T], F32)
            bb = ar.alloc([T], F32)
            hh = [ar.alloc([T], F32) for _ in range(2)]
            byg = ar.alloc([T], BF16)
            ybo = ar.alloc([T], BF16)
            dma('pool', lw[:, 0], IN["lru_w_a"][l].rearrange("d n c e -> c d n e"), writes=['lw'])
            dma('pool', lw[:, 1], IN["lru_w_x"][l].rearrange("d n c e -> c d n e"), writes=['lw'])
            segs = [(0, TC), (TC, TL)]
            for n_ in range(8):
                dma('sync', bxt, BX[n_], reads=['bxt'], writes=['bxt'])
                dma('sync', byg, BYG[n_], reads=['byg'], writes=['byg'])
                cw = [FT(l, F_CW + k * 8 + n_) for k in range(4)]
                cb = FT(l, F_CB + n_)
                for (s0, sn) in segs:
                    ts('dve', xc[:, s0:s0 + sn], bxt[:, s0:s0 + sn], cw[2], cb, ALU.mult, ALU.add, reads=['bxt', 'featT'], writes=['xc'])
                    stt(xc[:, s0 + 2:s0 + sn], bxt[:, s0:s0 + sn - 2], cw[0], xc[:, s0 + 2:s0 + sn], ALU.mult, ALU.add, reads=['bxt', 'xc'], writes=['xc'])
                    stt(xc[:, s0 + 1:s0 + sn], bxt[:, s0:s0 + sn - 1], cw[1], xc[:, s0 + 1:s0 + sn], ALU.mult, ALU.add, reads=['bxt', 'xc'], writes=['xc'])
                    stt(xc[:, s0:s0 + sn - 1], bxt[:, s0 + 1:s0 + sn], cw[3], xc[:, s0:s0 + sn - 1], ALU.mult, ALU.add, reads=['bxt', 'xc'], writes=['xc'])
                cp('pool', xcb, xc, reads=['xc'], writes=['xcb'])
                for d_ in range(2):
                    ba = FT(l, F_BA + d_ * 8 + n_)
                    bx_ = FT(l, F_BX + d_ * 8 + n_)
                    s8 = lruS[:, l, 0, d_ * 8 + n_:d_ * 8 + n_ + 1]
                    s16 = lruS[:, l, 1, d_ * 8 + n_:d_ * 8 + n_ + 1]
                    for bi, (t0, n) in enumerate(BLKS):
                        pa, px = (bi % 2) * 2, (bi % 2) * 2 + 1
                        mms(bank(pa, n), [(lw[:, 0, d_, n_, :], xcb[:, t0:t0 + n])], reads=['lw', 'xcb'], writes=[PS(pa)])
                        mms(bank(px, n), [(lw[:, 1, d_, n_, :], xcb[:, t0:t0 + n])], reads=['lw', 'xcb'], writes=[PS(px)])
                        act(rr[:, t0:t0 + n], bank(pa, n), AF.Sigmoid, reads=[PS(pa), 'featT'], writes=['rr'], bias=ba)
                        act(ii[:, t0:t0 + n], bank(px, n), AF.Sigmoid, reads=[PS(px), 'featT'], writes=['ii'], bias=bx_)
                    act(aa, rr, AF.Exp, reads=['rr', 'lruS'], writes=['aa'], scale=s8)
                    act(bb, rr, AF.Exp, reads=['rr', 'lruS'], writes=['bb'], scale=s16)
                    ts('pool', bb, bb, -1.0, 1.0, ALU.mult, ALU.add, reads=['bb'], writes=['bb'])
                    act(bb, bb, AF.Sqrt, reads=['bb'], writes=['bb'])
                    tt('dve', bb, bb, ii, ALU.mult, reads=['bb', 'ii'], writes=['bb'])
                    tt('dve', bb, bb, xc, ALU.mult, reads=['bb', 'xc'], writes=['bb'])
                    hd = hh[d_]
                    if d_ == 0:
                        S.add('dve', lambda e, hd=hd: e.tensor_tensor_scan(out=hd, data0=aa, data1=bb, initial=0.0, op0=ALU.mult, op1=ALU.add),
                              reads=['aa', 'bb'], writes=[('hh', d_)])
                    else:
                        S.add('dve', lambda e, hd=hd: e.tensor_tensor_scan(out=hd[:, TC - 1::-1] if False else hd[:, 0:TC][:, ::-1], data0=aa[:, 0:TC][:, ::-1],
                                                                            data1=bb[:, 0:TC][:, ::-1], initial=0.0, op0=ALU.mult, op1=ALU.add),
                              reads=['aa', 'bb'], writes=[('hh', d_)])
                        S.add('dve', lambda e, hd=hd: e.tensor_tensor_scan(out=hd[:, TC:T][:, ::-1], data0=aa[:, TC:T][:, ::-1],
                                                                            data1=bb[:, TC:T][:, ::-1], initial=hd[:, 0:1], op0=ALU.mult, op1=ALU.add),
                              reads=['aa', 'bb', ('hh', d_)], writes=[('hh', d_)])
                tt('pool', hh[0], hh[0], hh[1], ALU.add, reads=[('hh', 0), ('hh', 1)], writes=[('hh', 0)])
                tt('dve', ybo, hh[0], byg, ALU.mult, reads=[('hh', 0), 'byg'], writes=['ybo'])
                dma('sync', YB[n_], ybo, reads=['ybo'], writes=[U('YB')])
            S.barrier()
            ar.release()
            if phases is not None and 4 not in phases:
                continue

            ar.mark()
            wbr = ar.alloc([3, 8, D], BF16)
            yT = [ar.alloc([3, 8, 512], BF16) for _ in range(2)]
            gt = [ar.alloc([3, 512], BF16) for _ in range(2)]
            mst = [ar.alloc([KD, 512], BF16) for _ in range(2)]
            mtmp = [ar.alloc([2, 512], F32) for _ in range(2)]
            for bi_, nm in enumerate(["w_branch_a", "w_branch_b", "w_branch_c"]):
                dma('pool', wbr[:, bi_], IN[nm][l].rearrange("(k p) c -> p k c", p=128), writes=['wbr'])
            mblocks = BLKS if not last else BLKS[1:]
            for bi, (t0, n) in enumerate(mblocks):
                yi = bi % 2
                for bi_, Ysrc in enumerate([YA, YB, YC]):
                    dma('sync', yT[yi][:, bi_, :, 0:n], Ysrc[:, :, t0:t0 + n].rearrange("c p t -> p c t"), reads=[('yT', yi)], writes=[('yT', yi)])
                for dc in range(KD):
                    gi = dc % 2
                    dma('sync', gt[gi][:, :, 0:n], G[dc:48:16, :, t0:t0 + n].rearrange("c p t -> p c t"), reads=[('gt', gi)], writes=[('gt', gi)])
                    pbs = [(dc % 2) * 3 + b_ for b_ in range(3)]
                    for b_ in range(3):
                        mms(bank(pbs[b_], n), [(wbr[:, b_, k, dc * 128:(dc + 1) * 128], yT[yi][:, b_, k, 0:n]) for k in range(8)],
                            reads=['wbr', ('yT', yi)], writes=[PS(pbs[b_])])
                    tm = mtmp[gi]
                    tt('dve', tm[:, 0, 0:n], bank(pbs[0], n), gt[gi][:, 0, 0:n], ALU.mult, reads=[PS(pbs[0]), ('gt', gi)], writes=[('tm0', gi)])
                    tt('dve', tm[:, 1, 0:n], bank(pbs[1], n), gt[gi][:, 1, 0:n], ALU.mult, reads=[PS(pbs[1]), ('gt', gi)], writes=[('tm1', gi)])
                    tt('pool', tm[:, 0, 0:n], tm[:, 0, 0:n], tm[:, 1, 0:n], ALU.add, reads=[('tm0', gi), ('tm1', gi)], writes=[('tm0', gi)])
                    tt('dve', tm[:, 1, 0:n], bank(pbs[2], n), gt[gi][:, 2, 0:n], ALU.mult, reads=[PS(pbs[2]), ('gt', gi)], writes=[('tm1', gi)])
                    tt('pool', mst[yi][:, dc, 0:n], tm[:, 0, 0:n], tm[:, 1, 0:n], ALU.add, reads=[('tm0', gi), ('tm1', gi)], writes=[('mst', yi)])
                dma('sync', MT[:, :, t0:t0 + n].rearrange("c p t -> p c t"), mst[yi][:, :, 0:n], reads=[('mst', yi)], writes=[U('MT')])
            S.barrier()
            ar.release()

            ar.mark()
            wout = ar.alloc([KD, D], BF16)
            wr = ar.alloc([KD, 36], BF16)
            g1b = ar.alloc([2, D], F32)
            rbias = ar.alloc([36], F32)
            mTt = [ar.alloc([KD, 128], BF16) for _ in range(2)]
            hb2 = [ar.alloc([D], F32) for _ in range(2)]
            ytmp = ar.alloc([D], F32)
            junk = ar.alloc([D], BF16)
            xn2 = [ar.alloc([D], BF16) for _ in range(2)]
            ss2 = [ar.alloc([4], F32) for _ in range(2)]
            vTt = [ar.alloc([KD, 128], BF16) for _ in range(2)]
            rt = ar.alloc([96], F32)
            cmb = ar.alloc([NEXP], F32)
            cmbT = [ar.alloc([128], F32) for _ in range(2)]
            dma('pool', wout, IN["w_out"][l].rearrange("(k p) c -> p k c", p=128), writes=['wout'])
            dma('pool', wr[:, :, 0:4], IN["w_group"][l].rearrange("(k p) c -> p k c", p=128), writes=['wr'])
            dma('pool', wr[:, :, 4:36], IN["w_route"][l].rearrange("(k p) c -> p k c", p=128), writes=['wr'])
            dma('sync', g1b[:, 0, :], GATE[l, 0:1, 0, :].partition_broadcast(128), writes=['g1b'])
            dma('sync', g1b[:, 1, :], GATE[l, 1:2, 0, :].partition_broadcast(128), writes=['g1b'])
            dma('sync', rbias[:, 0:4], IN["b_group"][l:l + 1, :].partition_broadcast(128), writes=['rbias'])
            dma('sync', rbias[:, 4:36], IN["b_route"][l:l + 1, :].partition_broadcast(128), writes=['rbias'])
            tiles = list(range(NT)) if not last else list(range(2, NT))
            def A4(ti):
                t = tiles[ti]
                i = ti % 2
                r = 1 if t < 2 else 0
                dma('sync', mTt[i], MT[:, :, t * 128:(t + 1) * 128].rearrange("c p t -> p c t"), reads=[('mTt', i)], writes=[('mTt', i)])
                dma('sync', hb2[i], hsrc(l, t), reads=[('hb2', i)], writes=[('hb2', i)])
                for cb_ in range(4):
                    mms(bank(cb_), [(mTt[i][:, k, :], wout[:, k, cb_ * 512:(cb_ + 1) * 512]) for k in range(KD)],
                        reads=[('mTt', i), 'wout'], writes=[PS(cb_)])
                    tt('dve', ytmp[:, cb_ * 512:(cb_ + 1) * 512], bank(cb_), g1b[:, r, cb_ * 512:(cb_ + 1) * 512], ALU.mult,
                       reads=[PS(cb_), 'g1b'], writes=['ytmp'])
                tt('pool', hb2[i], hb2[i], ytmp, ALU.add, reads=[('hb2', i), 'ytmp'], writes=[('hb2', i)])
                dma('sync', H[t * 128:(t + 1) * 128, :], hb2[i], reads=[('hb2', i)], writes=[('H', t)])
                nm_A(hb2[i], ('hb2', i), junk, ss2[i], xn2[i], ('n2', i))

            def B4(ti):
                t = tiles[ti]
                i = ti % 2
                nm_B(l, t, 2, lambda c, i=i: (vTt[i][:, c, :], ('vTt', i)), xn2[i], pst_view(4), [PS(4), PS(5)], ('n2', i))
                dma('sync', VT[:, :, t * 128:(t + 1) * 128].rearrange("c p t -> p c t"), vTt[i], reads=[('vTt', i)], writes=[U('VT')])
                router(ti)

            def router(ti):
                t = tiles[ti]
                i = ti % 2
                mms(bank(6, 36), [(vTt[i][:, k, :], wr[:, k, :]) for k in range(KD)], reads=[('vTt', i), 'wr'], writes=[PS(6)])
                lg = rt[:, 0:36]
                tt('dve', lg, bank(6, 36), rbias, ALU.add, reads=[PS(6), 'rbias'], writes=['rt'])
                gl_, el_ = rt[:, 0:4], rt[:, 4:36].rearrange("p (g e) -> p g e", e=8)
                gmax, oneh, gex, gsum = rt[:, 36:37], rt[:, 40:44], rt[:, 44:48], rt[:, 48:49]
                els, m8, sel, dif = rt[:, 52:60], rt[:, 60:68], rt[:, 68:76], rt[:, 76:78]
                R_ = ['rt']
                S.add('dve', lambda e, gmax=gmax, gl_=gl_: e.tensor_reduce(out=gmax, in_=gl_, axis=AX.X, op=ALU.max), reads=R_, writes=R_)
                ts('dve', oneh, gl_, gmax, None, ALU.is_equal, None, reads=R_, writes=R_)
                ts('dve', gex, gl_, gmax, None, ALU.subtract, None, reads=R_, writes=R_)
                act(gex, gex, AF.Exp, reads=R_, writes=R_)
                S.add('dve', lambda e, gsum=gsum, gex=gex: e.tensor_reduce(out=gsum, in_=gex, axis=AX.X, op=ALU.add), reads=R_, writes=R_)
                recip(gsum, gsum, reads=R_, writes=R_)
                ts('dve', els, el_[:, 0, :], oneh[:, 0:1], None, ALU.mult, None, reads=R_, writes=R_)
                for g_ in range(1, 4):
                    stt(els, el_[:, g_, :], oneh[:, g_:g_ + 1], els, ALU.mult, ALU.add, reads=R_, writes=R_)
                S.add('dve', lambda e, m8=m8, els=els: e.max(out=m8, in_=els), reads=R_, writes=R_)
                tt('dve', dif[:, 1:2], m8[:, 1:2], m8[:, 0:1], ALU.subtract, reads=R_, writes=R_)
                act(dif[:, 1:2], dif[:, 1:2], AF.Exp, reads=R_, writes=R_)
                ts('dve', dif[:, 0:1], dif[:, 1:2], 1.0, None, ALU.add, None, reads=R_, writes=R_)
                recip(dif[:, 0:1], dif[:, 0:1], reads=R_, writes=R_)
                tt('dve', dif[:, 1:2], dif[:, 1:2], dif[:, 0:1], ALU.mult, reads=R_, writes=R_)
                ts('dve', dif, dif, gsum, None, ALU.mult, None, reads=R_, writes=R_)
                ts('dve', sel, els, m8[:, 0:1], dif[:, 0:1], ALU.is_equal, ALU.mult, reads=R_, writes=R_)
                ts('dve', m8[:, 2:8] if False else rt[:, 80:88], els, m8[:, 1:2], dif[:, 1:2], ALU.is_equal, ALU.mult, reads=R_, writes=R_)
                tt('dve', sel, sel, rt[:, 80:88], ALU.add, reads=R_, writes=R_)
                cm = cmb.rearrange("p (g e) -> p g e", e=8)
                for g_ in range(4):
                    ts('dve', cm[:, g_, :], sel, oneh[:, g_:g_ + 1], None, ALU.mult, None, reads=R_, writes=['cmb'])
                transposes([(bank(7, 128, 32), cmb)], ident_f, reads=['cmb', 'ident_f'], writes=[PS(7)])
                cp('dve', cmbT[i][0:32, :], bank(7, 128, 32), reads=[PS(7)], writes=[('cmbT', i)])
                dma('sync', CMB[:, t * 128:(t + 1) * 128], cmbT[i][0:32, :], reads=[('cmbT', i)], writes=[U('CMB')])

            A4(0)
            for ti in range(len(tiles)):
                if ti + 1 < len(tiles):
                    A4(ti + 1)
                B4(ti)
            S.barrier()
            ar.release()
            if phases is not None and 5 not in phases:
                continue

            mb = [(0, 768), (768, 768), (1536, 768)] if not last else [(256, 768), (1024, 768), (1792, 512)]
            ar.mark()
            BN = 768
            vTb_ = ar.alloc([KD, BN], BF16)
            acc = ar.alloc([BN // 128, D], F32)
            w1t = [ar.alloc([KD, DEXP], BF16) for _ in range(2)]
            w3t = [ar.alloc([KD, DEXP], BF16) for _ in range(2)]
            w2t = ar.alloc([4, D], BF16)
            hT_ = [ar.alloc([4, BN], BF16) for _ in range(2)]
            cbt_ = [ar.alloc([BN], F32) for _ in range(2)]
            s1_ = [ar.alloc([512], F32) for _ in range(2)]
            g2b = ar.alloc([D], F32)
            hb3 = ar.alloc([D], F32)
            k5 = {'p': 0, 'o': 0, 'e': 0}
            g2cur = [None]
            for (b0, bn) in mb:
                ntt = bn // 128
                nsb = 2 if bn > 512 else 1
                sbn = bn // nsb
                vTb = vTb_[:, :, 0:bn]
                dma('sync', vTb, VT[:, :, b0:b0 + bn].rearrange("c p t -> p c t"), reads=['vTb'], writes=['vTb'])
                S.add('pool', lambda e, ntt=ntt: e.memset(acc[:, 0:ntt, :], 0.0), reads=[('acc', i_) for i_ in range(ntt)],
                      writes=[('acc', i_) for i_ in range(ntt)])
                for ex in range(NEXP):
                    wi = k5['e'] % 2
                    k5['e'] += 1
                    hT = hT_[wi]
                    cbt = cbt_[wi]
                    dma('pool', w1t[wi], IN["w1"][l, ex].rearrange("(k p) c -> p k c", p=128), reads=[('w1', wi)], writes=[('w1', wi)])
                    dma('pool', w3t[wi], IN["w3"][l, ex].rearrange("(k p) c -> p k c", p=128), reads=[('w3', wi)], writes=[('w3', wi)])
                    dma('pool', w2t, IN["w2"][l, ex].rearrange("(k p) c -> p k c", p=128), reads=['w2'], writes=['w2'])
                    dma('sync', cbt[:, 0:bn], CMB[ex:ex + 1, b0:b0 + bn].partition_broadcast(128), reads=[('cbt', wi)], writes=[('cbt', wi)])
                    for sb_ in range(nsb):
                        q0 = sb_ * sbn
                        for dc in range(4):
                            pi = k5['p'] % 2
                            k5['p'] += 1
                            p1, p3 = pi * 2, pi * 2 + 1
                            s1 = s1_[pi][:, 0:sbn]
                            mms(bank(p1, sbn), [(w1t[wi][:, k, dc * 128:(dc + 1) * 128], vTb[:, k, q0:q0 + sbn]) for k in range(KD)],
                                reads=[('w1', wi), 'vTb'], writes=[PS(p1)])
                            mms(bank(p3, sbn), [(w3t[wi][:, k, dc * 128:(dc + 1) * 128], vTb[:, k, q0:q0 + sbn]) for k in range(KD)],
                                reads=[('w3', wi), 'vTb'], writes=[PS(p3)])
                            act(s1, bank(p1, sbn), AF.Silu, reads=[PS(p1)], writes=[('s1', pi)])
                            tt('dve', s1, s1, bank(p3, sbn), ALU.mult, reads=[('s1', pi), PS(p3)], writes=[('s1', pi)])
                            tt('dve', hT[:, dc, q0:q0 + sbn], s1, cbt[:, q0:q0 + sbn], ALU.mult,
                               reads=[('s1', pi), ('cbt', wi)], writes=[('hT', wi)])
                    for tt_ in range(ntt):
                        for cb_ in range(4):
                            po = 4 + k5['o'] % 4
                            k5['o'] += 1
                            mms(bank(po), [(hT[:, dc, tt_ * 128:(tt_ + 1) * 128], w2t[:, dc, cb_ * 512:(cb_ + 1) * 512]) for dc in range(4)],
                                reads=[('hT', wi), 'w2'], writes=[PS(po)])
                            tt('dve', acc[:, tt_, cb_ * 512:(cb_ + 1) * 512], acc[:, tt_, cb_ * 512:(cb_ + 1) * 512], bank(po), ALU.add,
                               reads=[PS(po), ('acc', tt_)], writes=[('acc', tt_)])
                for tt_ in range(ntt):
                    t = b0 // 128 + tt_
                    r = 1 if t < 2 else 0
                    if g2cur[0] != r:
                        dma('sync', g2b, GATE[l, r:r + 1, 1, :].partition_broadcast(128), reads=['g2b'], writes=['g2b'])
                        g2cur[0] = r
                    dma('sync', hb3, H[t * 128:(t + 1) * 128, :], reads=['hb3', ('H', t)], writes=['hb3'])
                    tt('dve', acc[:, tt_, :], acc[:, tt_, :], g2b, ALU.mult, reads=[('acc', tt_), 'g2b'], writes=[('acc', tt_)])
                    tt('dve', hb3, hb3, acc[:, tt_, :], ALU.add, reads=['hb3', ('acc', tt_)], writes=['hb3'])
                    if last:
                        dma('sync', OUT[(t - 2) * 128:(t - 1) * 128, :], hb3, reads=['hb3'], writes=[U('OUT')])
                    else:
                        dma('sync', H[t * 128:(t + 1) * 128, :], hb3, reads=['hb3'], writes=[('H', t)])
            S.barrier()
            ar.release()
        return

    with ExitStack() as st:
        try:
            body(st)
        except _Stop:
            pass
        S.emit(nc, st)
    return nc


def host_constants():
    ident = np.eye(128, dtype=np.float32)
    rotm = np.zeros((128, 128), np.float32)
    for j in range(128):
        blk = (j // 32) % 2
        if blk == 0:
            rotm[j + 32, j] = -1.0
        else:
            rotm[j - 32, j] = 1.0
    n_rows = TL // 64
    row = np.repeat(np.arange(n_rows), 64).astype(np.float32)
    col = np.tile(np.arange(64), n_rows).astype(np.float32)
    half = HD // 2
    inv = (1.0 / (np.float32(10000.0) ** (np.arange(0, half, 2, dtype=np.float32) / np.float32(half)))).astype(np.float32)
    ang_r = row[:, None] * inv
    ang_c = col[:, None] * inv
    ang = np.concatenate([ang_r, ang_r, ang_c, ang_c], axis=-1).astype(np.float32)
    cosT = np.ones((128, T), np.float32)
    sinT = np.zeros((128, T), np.float32)
    cosT[:, TC:] = np.cos(ang).T
    sinT[:, TC:] = np.sin(ang).T
    return {"ident": ident, "rotm": rotm, "cosT": np.ascontiguousarray(cosT), "sinT": np.ascontiguousarray(sinT)}


_NC_CACHE = {}


def kernel(**inputs):
    n = 8
    if 'nc' not in _NC_CACHE:
        _NC_CACHE['nc'] = build()
    nc = _NC_CACHE['nc']
    consts = host_constants()
    shared = {k: np.ascontiguousarray(np.asarray(inputs[k], dtype=np.float32)) for k in WEIGHT_NAMES}
    shared.update(consts)
    x = np.asarray(inputs["x"], dtype=np.float32)
    c = np.asarray(inputs["c"], dtype=np.float32)
    ctx = np.asarray(inputs["ctx"], dtype=np.float32)
    c_ctx = np.asarray(inputs["c_ctx"], dtype=np.float32)
    in_maps = []
    for b in range(n):
        m = dict(shared)
        m["x"] = np.ascontiguousarray(x[b])
        m["ctx"] = np.ascontiguousarray(ctx[b])
        m["c2"] = np.ascontiguousarray(np.stack([c[b], c_ctx], axis=0))
        in_maps.append(m)
    res = run_bass_kernel_spmd(nc, in_maps, core_ids=list(range(n)))
    return np.stack([r["out"] for r in res.results], axis=0).astype(np.float32)
```
